# Optimizing a Trainium2 kernel written in Bass

```python
import jax, jax.numpy as jnp
from jax import lax
import numpy as np

D_MODEL = 2048
BATCH = 4
SEQ = 4096
DEPTH = 2

GRID_W = 64
CTX_LEN = 256
RMS_EPS = 1e-6

N_Q_HEADS = 8
N_KV_HEADS = 2
HEAD_DIM = 128
WINDOW = 128
ATTN_BLOCK = 128
ROPE_THETA = 10000.0
ROPE_AXIS_DIM = HEAD_DIM // 2

GLA_HEADS = 4
GLA_DK = 64
GLA_DV = 128
GLA_RANK = 16
GLA_TAU = 16.0
GLA_CHUNK = 16

LRU_WIDTH = 512
LRU_BLOCKS = 8
LRU_BLOCK_W = LRU_WIDTH // LRU_BLOCKS
LRU_C = 8.0
CONV_W = 4
CONV_LEFT = 2

ATTN_Q = N_Q_HEADS * HEAD_DIM
ATTN_KV = N_KV_HEADS * HEAD_DIM
GLA_QK = GLA_HEADS * GLA_DK
GLA_V = GLA_HEADS * GLA_DV
D_MIX = ATTN_Q + GLA_V + LRU_WIDTH
IN_SPLITS = (ATTN_Q, ATTN_KV, ATTN_KV, GLA_QK, GLA_QK, GLA_V, GLA_V, 2 * GLA_RANK, LRU_WIDTH, LRU_WIDTH)
D_IN = 4128

N_GROUPS = 4
EXPERTS_PER_GROUP = 8
N_EXPERTS = N_GROUPS * EXPERTS_PER_GROUP
TOP_K = 2
D_FF_EXPERT = 1024
MOE_BLOCK = 256

kernel_name = 'hybrid_dit_swa_gla_rglru_hmoe'


def rms_norm(x, g):
    x32 = x.astype(jnp.float32)
    y = x32 * lax.rsqrt(jnp.mean(x32 * x32, axis=-1, keepdims=True) + RMS_EPS)
    return (y * g.astype(jnp.float32)).astype(x.dtype)


def modulate(h, shift, scale):
    return h * (1 + scale) + shift


def rev(a):
    return jnp.flip(a, axis=1)


def split_cols(z):
    cuts = np.cumsum(IN_SPLITS)[:-1].tolist()
    return jnp.split(z, cuts, axis=-1)


def axial_rope_angles(n_tokens):
    rows = n_tokens // GRID_W
    row = jnp.repeat(jnp.arange(rows, dtype=jnp.float32), GRID_W)
    col = jnp.tile(jnp.arange(GRID_W, dtype=jnp.float32), rows)
    n_freq = ROPE_AXIS_DIM // 2
    inv = ROPE_THETA ** (-jnp.arange(n_freq, dtype=jnp.float32) / n_freq)
    ang = jnp.concatenate([row[:, None] * inv, col[:, None] * inv], axis=-1)
    return jnp.cos(ang), jnp.sin(ang)


def apply_axial_rope(x, cos, sin):
    xp = x.astype(jnp.float32).reshape(x.shape[:-1] + (HEAD_DIM // 2, 2))
    x1, x2 = xp[..., 0], xp[..., 1]
    cs, sn = cos[None, :, None, :], sin[None, :, None, :]
    out = jnp.stack([x1 * cs - x2 * sn, x1 * sn + x2 * cs], axis=-1)
    return out.reshape(x.shape).astype(x.dtype)


def windowed_attention(q, k, v, k_ctx, v_ctx, sink):
    B, S, Hq, hd = q.shape
    G = Hq // N_KV_HEADS
    nb = S // ATTN_BLOCK
    C = k_ctx.shape[1]
    nk = 3 * ATTN_BLOCK
    scale = HEAD_DIM ** -0.5
    qb = q.reshape(B, nb, ATTN_BLOCK, N_KV_HEADS, G, hd)
    pad = ((0, 0), (ATTN_BLOCK, ATTN_BLOCK), (0, 0), (0, 0))
    kp = jnp.pad(k, pad).reshape(B, nb + 2, ATTN_BLOCK, N_KV_HEADS, hd)
    vp = jnp.pad(v, pad).reshape(B, nb + 2, ATTN_BLOCK, N_KV_HEADS, hd)
    k_band = jnp.concatenate([kp[:, :-2], kp[:, 1:-1], kp[:, 2:]], axis=2)
    v_band = jnp.concatenate([vp[:, :-2], vp[:, 1:-1], vp[:, 2:]], axis=2)
    s_loc = jnp.einsum('bnqhgd,bnkhd->bnhgqk', qb, k_band).astype(jnp.float32) * scale
    s_ctx = jnp.einsum('bnqhgd,bchd->bnhgqc', qb, k_ctx).astype(jnp.float32) * scale
    blk = jnp.arange(nb)[:, None, None]
    qpos = blk * ATTN_BLOCK + jnp.arange(ATTN_BLOCK)[None, :, None]
    kpos = (blk - 1) * ATTN_BLOCK + jnp.arange(nk)[None, None, :]
    valid = (jnp.abs(kpos - qpos) <= WINDOW) & (kpos >= 0) & (kpos < S)
    s_loc = jnp.where(valid[None, :, None, None], s_loc, -jnp.inf)
    sink_col = jnp.broadcast_to(sink.astype(jnp.float32).reshape(N_KV_HEADS, G)[None, None, :, :, None, None], s_loc.shape[:-1] + (1,))
    p = jax.nn.softmax(jnp.concatenate([s_loc, s_ctx, sink_col], axis=-1), axis=-1).astype(v.dtype)
    o = jnp.einsum('bnhgqk,bnkhd->bnqhgd', p[..., :nk], v_band) + jnp.einsum('bnhgqc,bchd->bnqhgd', p[..., nk:nk + C], v_ctx)
    return o.reshape(B, S, Hq * hd)


def context_attention(q, k, v, sink):
    B, C, Hq, hd = q.shape
    G = Hq // N_KV_HEADS
    qg = q.reshape(B, C, N_KV_HEADS, G, hd)
    s = jnp.einsum('bqhgd,bkhd->bhgqk', qg, k).astype(jnp.float32) * (HEAD_DIM ** -0.5)
    sink_col = jnp.broadcast_to(sink.astype(jnp.float32).reshape(N_KV_HEADS, G)[None, :, :, None, None], s.shape[:-1] + (1,))
    p = jax.nn.softmax(jnp.concatenate([s, sink_col], axis=-1), axis=-1)[..., :C].astype(v.dtype)
    o = jnp.einsum('bhgqk,bkhd->bqhgd', p, v)
    return o.reshape(B, C, Hq * hd)


def gla_qkv(gq, gk, gv):
    B, T, _ = gq.shape
    q = gq.reshape(B, T, GLA_HEADS, GLA_DK) * (GLA_DK ** -0.5)
    k = gk.reshape(B, T, GLA_HEADS, GLA_DK)
    v = gv.reshape(B, T, GLA_HEADS, GLA_DV)
    return q, k, v


def gla_log_decay(z_low, w2, b):
    z = (z_low @ w2 + b).astype(jnp.float32)
    return (jax.nn.log_sigmoid(z) / GLA_TAU).reshape(z.shape[:-1] + (GLA_HEADS, GLA_DK))


def gla_chunked(q, k, v, log_a, s0):
    B, T, H, dk = q.shape
    dv = v.shape[-1]
    L = GLA_CHUNK
    nc = T // L
    qc = q.astype(jnp.float32).reshape(B, nc, L, H, dk)
    kc = k.astype(jnp.float32).reshape(B, nc, L, H, dk)
    vc = v.astype(jnp.float32).reshape(B, nc, L, H, dv)
    b = jnp.cumsum(log_a.reshape(B, nc, L, H, dk), axis=2)
    b_last = b[:, :, -1]
    tri = jnp.tril(jnp.ones((L, L), dtype=bool))
    diff = b[:, :, :, None] - b[:, :, None, :]
    decay = jnp.exp(jnp.where(tri[None, None, :, :, None, None], diff, -jnp.inf))
    A = jnp.einsum('bntshd,bnthd,bnshd->bnhts', decay, qc, kc)
    o_intra = jnp.einsum('bnhts,bnshv->bnthv', A, vc)
    contrib = jnp.einsum('bnshd,bnshv->bnhdv', kc * jnp.exp(b_last[:, :, None] - b), vc)
    a_tot = jnp.exp(b_last)

    def step(s, inp):
        a, u = inp
        return a[..., None] * s + u, s

    s_final, s_before = lax.scan(step, s0, (jnp.moveaxis(a_tot, 1, 0), jnp.moveaxis(contrib, 1, 0)))
    s_before = jnp.moveaxis(s_before, 0, 1)
    o_inter = jnp.einsum('bnthd,bnhdv->bnthv', qc * jnp.exp(b), s_before)
    return (o_intra + o_inter).reshape(B, T, H, dv), s_final


def gla_output(o, r, g):
    y = rms_norm(o, g).reshape(o.shape[:2] + (GLA_V,))
    return (y * jax.nn.silu(r.astype(jnp.float32))).astype(r.dtype)


def short_conv(x, w, b):
    T = x.shape[1]
    xp = jnp.pad(x, ((0, 0), (CONV_LEFT, CONV_W - 1 - CONV_LEFT), (0, 0)))
    y = b
    for j in range(CONV_W):
        y = y + xp[:, j:j + T] * w[j]
    return y


def lru_gates(x, wa, ba, wx, bx, lam):
    B, T, _ = x.shape
    xb = x.reshape(B, T, LRU_BLOCKS, LRU_BLOCK_W)
    r = jax.nn.sigmoid((jnp.einsum('btnc,ncd->btnd', xb, wa).reshape(B, T, LRU_WIDTH) + ba).astype(jnp.float32))
    i = jax.nn.sigmoid((jnp.einsum('btnc,ncd->btnd', xb, wx).reshape(B, T, LRU_WIDTH) + bx).astype(jnp.float32))
    log_a = -LRU_C * jax.nn.softplus(-lam.astype(jnp.float32)) * r
    u = jnp.sqrt(-jnp.expm1(2.0 * log_a)) * (i * x.astype(jnp.float32))
    return log_a, u


def _affine_combine(e1, e2):
    a1, b1 = e1
    a2, b2 = e2
    return a1 * a2, a2 * b1 + b2


def linear_scan(log_a, u, h0):
    A, Bv = lax.associative_scan(_affine_combine, (jnp.exp(log_a), u), axis=1)
    h = Bv + A * h0[:, None, :]
    return h, h[:, -1]


def lru_output(h, gate):
    return (h * jax.nn.gelu(gate.astype(jnp.float32))).astype(gate.dtype)


def hybrid_mixer(u_l, u_c, cos, sin, w_in, sink, gla_w2, gla_b, gla_g, conv_w, conv_b, lru_wa, lru_ba, lru_wx, lru_bx, lru_lam, w_out, need_ctx_out):
    B, S, _ = u_l.shape
    C = u_c.shape[1]
    aq_l, ak_l, av_l, gq_l, gk_l, gv_l, gr_l, gz_l, lx_l, lg_l = split_cols(u_l @ w_in)
    aq_c, ak_c, av_c, gq_c, gk_c, gv_c, gr_c, gz_c, lx_c, lg_c = split_cols(u_c @ w_in)

    q_l = apply_axial_rope(aq_l.reshape(B, S, N_Q_HEADS, HEAD_DIM), cos, sin)
    k_l = apply_axial_rope(ak_l.reshape(B, S, N_KV_HEADS, HEAD_DIM), cos, sin)
    v_l = av_l.reshape(B, S, N_KV_HEADS, HEAD_DIM)
    k_c = ak_c.reshape(B, C, N_KV_HEADS, HEAD_DIM)
    v_c = av_c.reshape(B, C, N_KV_HEADS, HEAD_DIM)
    attn_l = windowed_attention(q_l, k_l, v_l, k_c, v_c, sink)

    qg_l, kg_l, vg_l = gla_qkv(gq_l, gk_l, gv_l)
    qg_c, kg_c, vg_c = gla_qkv(gq_c, gk_c, gv_c)
    la_l_f = gla_log_decay(gz_l[..., :GLA_RANK], gla_w2[0], gla_b[0])
    la_l_b = gla_log_decay(gz_l[..., GLA_RANK:], gla_w2[1], gla_b[1])
    la_c_f = gla_log_decay(gz_c[..., :GLA_RANK], gla_w2[0], gla_b[0])
    la_c_b = gla_log_decay(gz_c[..., GLA_RANK:], gla_w2[1], gla_b[1])
    s_zero = jnp.zeros((B, GLA_HEADS, GLA_DK, GLA_DV), jnp.float32)
    oc_f, st_f = gla_chunked(qg_c, kg_c, vg_c, la_c_f, s_zero)
    oc_b, st_b = gla_chunked(rev(qg_c), rev(kg_c), rev(vg_c), rev(la_c_b), s_zero)
    ol_f, _ = gla_chunked(qg_l, kg_l, vg_l, la_l_f, st_f)
    ol_b, _ = gla_chunked(rev(qg_l), rev(kg_l), rev(vg_l), rev(la_l_b), st_b)
    gla_l = gla_output(ol_f + rev(ol_b), gr_l, gla_g)

    xc_l = short_conv(lx_l, conv_w, conv_b)
    xc_c = short_conv(lx_c, conv_w, conv_b)
    h_zero = jnp.zeros((B, LRU_WIDTH), jnp.float32)
    lcf, ucf = lru_gates(xc_c, lru_wa[0], lru_ba[0], lru_wx[0], lru_bx[0], lru_lam[0])
    lcb, ucb = lru_gates(xc_c, lru_wa[1], lru_ba[1], lru_wx[1], lru_bx[1], lru_lam[1])
    llf, ulf = lru_gates(xc_l, lru_wa[0], lru_ba[0], lru_wx[0], lru_bx[0], lru_lam[0])
    llb, ulb = lru_gates(xc_l, lru_wa[1], lru_ba[1], lru_wx[1], lru_bx[1], lru_lam[1])
    hc_f, hs_f = linear_scan(lcf, ucf, h_zero)
    hc_b, hs_b = linear_scan(rev(lcb), rev(ucb), h_zero)
    hl_f, _ = linear_scan(llf, ulf, hs_f)
    hl_b, _ = linear_scan(rev(llb), rev(ulb), hs_b)
    lru_l = lru_output(hl_f + rev(hl_b), lg_l)

    y_l = jnp.concatenate([attn_l, gla_l, lru_l], axis=-1) @ w_out
    if not need_ctx_out:
        return y_l, None
    attn_c = context_attention(aq_c.reshape(B, C, N_Q_HEADS, HEAD_DIM), k_c, v_c, sink)
    gla_c = gla_output(oc_f + rev(oc_b), gr_c, gla_g)
    lru_c = lru_output(hc_f + rev(hc_b), lg_c)
    y_c = jnp.concatenate([attn_c, gla_c, lru_c], axis=-1) @ w_out
    return y_l, y_c


def expert_dispatch(h, experts, gates, w1, w3, w2):
    N, D = h.shape
    M = N * TOP_K
    flat_e = experts.reshape(M).astype(jnp.int32)
    flat_tok = jnp.arange(M, dtype=jnp.int32) // TOP_K
    flat_w = gates.reshape(M)
    order = jnp.argsort(flat_e)
    e_sorted = flat_e[order]
    tok_sorted = flat_tok[order]
    w_sorted = flat_w[order]
    counts = jnp.bincount(flat_e, length=N_EXPERTS).astype(jnp.int32)
    padded = (counts + MOE_BLOCK - 1) // MOE_BLOCK * MOE_BLOCK
    pad_end = jnp.cumsum(padded)
    pad_start = pad_end - padded
    start = jnp.cumsum(counts) - counts
    dest = pad_start[e_sorted] + (jnp.arange(M, dtype=jnp.int32) - start[e_sorted])
    n_blocks = -(-(M + N_EXPERTS * (MOE_BLOCK - 1)) // MOE_BLOCK)
    P = n_blocks * MOE_BLOCK
    src = jnp.full((P,), N, dtype=jnp.int32).at[dest].set(tok_sorted)
    w_pad = jnp.zeros((P,), jnp.float32).at[dest].set(w_sorted)
    h_ext = jnp.concatenate([h, jnp.zeros((1, D), h.dtype)], axis=0)
    xs = h_ext[src].reshape(n_blocks, MOE_BLOCK, D)
    blk_e = jnp.minimum(jnp.searchsorted(pad_end, jnp.arange(n_blocks, dtype=jnp.int32) * MOE_BLOCK, side='right'), N_EXPERTS - 1)

    def expert_block(args):
        xb, e = args
        return (jax.nn.silu(xb @ w1[e]) * (xb @ w3[e])) @ w2[e]

    ys = lax.map(expert_block, (xs, blk_e)).reshape(P, D)
    y = jax.ops.segment_sum(ys * w_pad[:, None].astype(ys.dtype), src, num_segments=N + 1)
    return y[:N]


def hierarchical_moe(h, wg, bg, we, be, w1, w3, w2):
    N, _ = h.shape
    pg = jax.nn.softmax((h @ wg).astype(jnp.float32) + bg.astype(jnp.float32), axis=-1)
    p_grp, grp = lax.top_k(pg, 1)
    le = ((h @ we).astype(jnp.float32) + be.astype(jnp.float32)).reshape(N, N_GROUPS, EXPERTS_PER_GROUP)
    le_sel = jnp.take_along_axis(le, grp[:, :, None], axis=1)[:, 0]
    w_top, e_top = lax.top_k(jax.nn.softmax(le_sel, axis=-1), TOP_K)
    w_top = w_top / jnp.sum(w_top, axis=-1, keepdims=True)
    gates = p_grp * w_top
    experts = grp * EXPERTS_PER_GROUP + e_top
    return expert_dispatch(h, experts, gates, w1, w3, w2)


def setup_inputs(seed: int = 0) -> dict:
    key = jax.random.key(seed)
    ks = jax.random.split(key, 32)
    f32 = jnp.float32
    D, L = D_MODEL, DEPTH

    def nrm(k, shape, scale):
        return jax.random.normal(k, shape, f32) * scale

    lam_u = jax.random.uniform(ks[19], (L, 2, LRU_WIDTH), f32, 0.9, 0.999)
    lam_s = lam_u ** (1.0 / LRU_C)
    return {
        'x': nrm(ks[0], (BATCH, SEQ, D), 1.0),
        'c': nrm(ks[1], (BATCH, D), 1.0),
        'ctx': nrm(ks[2], (BATCH, CTX_LEN, D), 1.0),
        'c_ctx': nrm(ks[3], (D,), 1.0),
        'ada_w': nrm(ks[4], (L, D, 6 * D), 0.5 * D ** -0.5),
        'ada_b': nrm(ks[5], (L, 6 * D), 0.02),
        'norm_mix_g': 1.0 + nrm(ks[6], (L, D), 0.02),
        'norm_ffn_g': 1.0 + nrm(ks[7], (L, D), 0.02),
        'w_in': nrm(ks[8], (L, D, D_IN), D ** -0.5),
        'attn_sink': nrm(ks[9], (L, N_Q_HEADS), 0.5),
        'gla_gate_w2': nrm(ks[10], (L, 2, GLA_RANK, GLA_QK), GLA_RANK ** -0.5),
        'gla_gate_b': nrm(ks[11], (L, 2, GLA_QK), 0.1),
        'gla_norm_g': 1.0 + nrm(ks[12], (L, GLA_DV), 0.02),
        'lru_conv_w': nrm(ks[13], (L, CONV_W, LRU_WIDTH), CONV_W ** -0.5),
        'lru_conv_b': nrm(ks[14], (L, LRU_WIDTH), 0.02),
        'lru_wa': nrm(ks[15], (L, 2, LRU_BLOCKS, LRU_BLOCK_W, LRU_BLOCK_W), LRU_BLOCK_W ** -0.5),
        'lru_ba': nrm(ks[16], (L, 2, LRU_WIDTH), 0.02),
        'lru_wx': nrm(ks[17], (L, 2, LRU_BLOCKS, LRU_BLOCK_W, LRU_BLOCK_W), LRU_BLOCK_W ** -0.5),
        'lru_bx': nrm(ks[18], (L, 2, LRU_WIDTH), 0.02),
        'lru_lambda': jnp.log(lam_s) - jnp.log1p(-lam_s),
        'w_out': nrm(ks[20], (L, D_MIX, D), D_MIX ** -0.5),
        'router_g_w': nrm(ks[21], (L, D, N_GROUPS), D ** -0.5),
        'router_g_b': nrm(ks[22], (L, N_GROUPS), 0.01),
        'router_e_w': nrm(ks[23], (L, D, N_EXPERTS), D ** -0.5),
        'router_e_b': nrm(ks[24], (L, N_EXPERTS), 0.01),
        'moe_w1': nrm(ks[25], (L, N_EXPERTS, D, D_FF_EXPERT), D ** -0.5),
        'moe_w3': nrm(ks[26], (L, N_EXPERTS, D, D_FF_EXPERT), D ** -0.5),
        'moe_w2': nrm(ks[27], (L, N_EXPERTS, D_FF_EXPERT, D), D_FF_EXPERT ** -0.5),
        'final_norm_g': 1.0 + nrm(ks[28], (D,), 0.02),
    }


def reference(x, c, ctx, c_ctx, ada_w, ada_b, norm_mix_g, norm_ffn_g, w_in, attn_sink, gla_gate_w2, gla_gate_b, gla_norm_g, lru_conv_w, lru_conv_b, lru_wa, lru_ba, lru_wx, lru_bx, lru_lambda, w_out, router_g_w, router_g_b, router_e_w, router_e_b, moe_w1, moe_w3, moe_w2, final_norm_g):
    B, S, D = x.shape
    C = ctx.shape[1]
    cos, sin = axial_rope_angles(S)
    cond_l = jax.nn.silu(c)
    cond_c = jax.nn.silu(c_ctx)
    h_lat, h_ctx = x, ctx
    for i in range(DEPTH):
        last = i == DEPTH - 1
        mods_l = [m[:, None, :] for m in jnp.split(cond_l @ ada_w[i] + ada_b[i], 6, axis=-1)]
        mods_c = jnp.split(cond_c @ ada_w[i] + ada_b[i], 6, axis=-1)
        sh1_l, sc1_l, g1_l, sh2_l, sc2_l, g2_l = mods_l
        sh1_c, sc1_c, g1_c, sh2_c, sc2_c, g2_c = mods_c
        u_l = modulate(rms_norm(h_lat, norm_mix_g[i]), sh1_l, sc1_l)
        u_c = modulate(rms_norm(h_ctx, norm_mix_g[i]), sh1_c, sc1_c)
        y_l, y_c = hybrid_mixer(u_l, u_c, cos, sin, w_in[i], attn_sink[i], gla_gate_w2[i], gla_gate_b[i], gla_norm_g[i], lru_conv_w[i], lru_conv_b[i], lru_wa[i], lru_ba[i], lru_wx[i], lru_bx[i], lru_lambda[i], w_out[i], not last)
        h_lat = h_lat + g1_l * y_l
        v_l = modulate(rms_norm(h_lat, norm_ffn_g[i]), sh2_l, sc2_l).reshape(B * S, D)
        moe_args = (router_g_w[i], router_g_b[i], router_e_w[i], router_e_b[i], moe_w1[i], moe_w3[i], moe_w2[i])
        if last:
            f_l = hierarchical_moe(v_l, *moe_args)
        else:
            h_ctx = h_ctx + g1_c * y_c
            v_c = modulate(rms_norm(h_ctx, norm_ffn_g[i]), sh2_c, sc2_c).reshape(B * C, D)
            f = hierarchical_moe(jnp.concatenate([v_l, v_c], axis=0), *moe_args)
            f_l = f[:B * S]
            h_ctx = h_ctx + g2_c * f[B * S:].reshape(B, C, D)
        h_lat = h_lat + g2_l * f_l.reshape(B, S, D)
    return rms_norm(h_lat, final_norm_g)
```

```python
import contextlib
import numpy as np
import ml_dtypes
import concourse.bass as bass
import concourse.mybir as mybir
from concourse.bass_utils import run_bass_kernel_spmd

F32 = mybir.dt.float32
BF16 = mybir.dt.bfloat16
AF = mybir.ActivationFunctionType
ALU = mybir.AluOpType
AX = mybir.AxisListType

D = 2048
NCTX = 256
NLAT = 4096
T = NCTX + NLAT
NTT = T // 128
DC = D // 128
OWN_LAT = 2048
OWN_CTX = 128
N_FM = 23
N_TM = 3
NCOLS = N_FM * 128 + N_TM * 512
EPS = 1e-6
import os
CUT = int(os.environ.get('KCUT', '0'))


class Prog:
    def __init__(self, nc, same_engine_sync=True):
        self.nc = nc
        self.ops = []
        self.same_engine_sync = same_engine_sync
        self.stack = contextlib.ExitStack()
        self.n_sb = 0

    def sb(self, shape, dtype, name=None, stack=None):
        self.n_sb += 1
        return (stack or self.stack).enter_context(self.nc.sbuf_tensor(f"s{self.n_sb}_" + (name or "t"), list(shape), dtype))

    def ps(self, shape, dtype=F32, name=None, stack=None):
        self.n_sb += 1
        return (stack or self.stack).enter_context(self.nc.psum_tensor(f"p{self.n_sb}_" + (name or "t"), list(shape), dtype))

    def add(self, eng, fn, reads=(), writes=(), dma=False, semkey=None):
        self.ops.append(dict(eng=eng, fn=fn, reads=tuple(reads), writes=tuple(writes), dma=dma, semkey=semkey))

    def fence(self):
        self.ops.append(dict(fence=True))

    def dma(self, eng, out, in_, reads=(), writes=(), semkey=None, **kw):
        if semkey is None:
            semkey = ("dma",) + (tuple(writes) if writes else tuple(reads))
        self.add(eng, lambda e: e.dma_start(out=out, in_=in_, **kw), reads, writes, dma=True, semkey=semkey)

    def emit(self):
        nc = self.nc
        ops = self.ops
        sems = {}
        counts, last_writer, readers, waited, per_eng, final_vals = {}, {}, {}, {}, {}, {}
        pending_fence = {}
        for i, op in enumerate(ops):
            if op.get("fence"):
                snap = dict(counts)
                for En in ("sync", "tensor", "vector", "scalar", "gpsimd"):
                    pending_fence[En] = snap
                continue
            E = op["eng"]
            deps = set()
            for k in op["reads"]:
                if k in last_writer:
                    deps.add(last_writer[k])
            for k in op["writes"]:
                if k in last_writer:
                    deps.add(last_writer[k])
                for r in readers.get(k, ()):
                    deps.add(r)
            waits = {}
            for j in deps:
                pj = ops[j]
                if (not pj["dma"]) and pj["eng"] == E and (not op["dma"]):
                    if E == "tensor" or not self.same_engine_sync:
                        continue
                sk, val = pj["sig"]
                if waited.get((E, sk), 0) >= val:
                    continue
                waits[sk] = max(waits.get(sk, 0), val)
            if E in pending_fence:
                for sk, val in pending_fence.pop(E).items():
                    if sk == ("eng", E) and E == "tensor":
                        continue
                    if waited.get((E, sk), 0) < val:
                        waits[sk] = max(waits.get(sk, 0), val)
            for sk, val in waits.items():
                waited[(E, sk)] = val
            if op["dma"]:
                sk = op["semkey"]
                counts[sk] = counts.get(sk, 0) + 16
            else:
                sk = ("eng", E)
                counts[sk] = counts.get(sk, 0) + 1
            op["sig"] = (sk, counts[sk])
            final_vals[sk] = counts[sk]
            op["waits"] = waits
            for k in op["writes"]:
                last_writer[k] = i
                readers[k] = []
            for k in op["reads"]:
                readers.setdefault(k, []).append(i)
            per_eng.setdefault(E, []).append(op)
        ops = [o for o in ops if not o.get("fence")]
        for o in ops:
            sk = o["sig"][0]
            if sk not in sems:
                sems[sk] = self.stack.enter_context(nc.semaphore(f"s{len(sems)}"))
        self.n_sems = len(sems)
        self.n_ops = len(ops)
        with nc.Block() as block:
            def make(Ename):
                def body(eng):
                    for op in per_eng.get(Ename, []):
                        for sk, val in op["waits"].items():
                            eng.wait_ge(sems[sk], val)
                        ins = op["fn"](eng)
                        ins.then_inc(sems[op["sig"][0]], 16 if op["dma"] else 1)
                    if Ename == "sync":
                        for sk, val in final_vals.items():
                            eng.wait_ge(sems[sk], val)
                return body
            for Ename in ("sync", "tensor", "vector", "scalar", "gpsimd"):
                getattr(block, Ename)(make(Ename))
        self.stack.close()


class Ring:
    def __init__(self, P, n, shape, dtype, name, psum=False, stack=None):
        self.bufs = []
        for i in range(n):
            t = (P.ps if psum else P.sb)(shape, dtype, name=f"{name}{i}", stack=stack)
            self.bufs.append((t, f"{name}{i}"))
        self.i = 0

    def next(self):
        b = self.bufs[self.i % len(self.bufs)]
        self.i += 1
        return b


def groups_of(n0, n1, g=512):
    out = []
    p = n0
    while p < n1:
        out.append((p, min(g, n1 - p)))
        p += g
    return out


def declare_dram(nc, last, debug=(), moe=True):
    dr = {}

    def inp(name, shape, dt=F32):
        dr[name] = nc.dram_tensor(name, list(shape), dt, kind="ExternalInput").ap()

    def scr(name, shape, dt):
        kind = "ExternalOutput" if name in debug else "Internal"
        dr[name] = nc.dram_tensor(name, list(shape), dt, kind=kind).ap()

    inp("hin", [T, D])
    inp("condT", [128, DC, 2])
    inp("ada_w", [D, 6 * D])
    inp("ada_b", [1, 6 * D])
    inp("gmixT", [128, DC])
    inp("gffnT", [128, DC])
    inp("w_in_p", [D, NCOLS])
    inp("ropeC", [128, T])
    inp("ropeS", [128, T])
    inp("pswap", [128, 128])
    inp("ident", [128, 128])
    inp("maskL", [128, 512])
    inp("maskR", [128, 512])
    inp("sink", [1, 8])
    inp("gw2p", [64, 256])
    inp("gnorm", [1, 128])
    inp("tri", [4, 128, 128])
    inp("gmask", [2, 128, 128])
    inp("convw", [128, 4, 5])
    inp("convb", [128, 4])
    inp("lru_w", [2, 2, 4, 128, 128])
    inp("lru_b", [128, 2, 2, 4])
    inp("lru_lam", [128, 2, 4])
    inp("w_out", [D, D])
    inp("wr", [D, 36])
    inp("br", [1, 36])
    if moe:
        inp("moe_w1", [32, D, 1024])
        inp("moe_w3", [32, D, 1024])
        inp("moe_w2", [32, 1024, D])
    inp("gfin", [1, D])
    scr("modrow", [2, 6 * D], F32)
    scr("qT", [8, 128, T], BF16)
    scr("kT", [2, 128, T], BF16)
    scr("vS", [T, 256], BF16)
    scr("gqT", [2, 128, T], BF16)
    scr("gkT", [2, 128, T], BF16)
    scr("gkS", [T, 256], BF16)
    scr("gvS", [T, 512], BF16)
    scr("grS", [T, 512], F32)
    scr("gzT", [64, T], F32)
    scr("lxT", [4, 128, T], F32)
    scr("lgT", [4, 128, T], F32)
    scr("mixT", [16, 128, T], BF16)
    nown = OWN_CTX + OWN_LAT
    scr("h1", [nown, D], F32)
    scr("vTs", [128, DC, nown], BF16)
    if last:
        dr["out"] = nc.dram_tensor("out", [OWN_LAT, D], F32, kind="ExternalOutput").ap()
    else:
        dr["out"] = nc.dram_tensor("out", [nown, D], F32, kind="ExternalOutput").ap()
    return dr


def phase_mods(P, nc, dr, pers):
    st = contextlib.ExitStack()
    condT = P.sb([128, DC, 2], F32, "condT", st)
    scl = P.sb([128, DC, 2], F32, "scl", st)
    gmix = P.sb([128, DC], F32, "gmixs", st)
    gffn = P.sb([128, DC], F32, "gffns", st)
    brow = P.sb([2, 2048], F32, "brow", st)
    grow_sb = P.sb([2, 2048], F32, "grow_sb", st)
    psT = [P.ps([2, 512], F32, f"psT{i}", st) for i in range(4)]
    wring = Ring(P, 4, [128, 2048], F32, "adaw", stack=st)
    modsT = pers["modsT"]

    P.dma("sync", condT[:], dr["condT"], writes=["condT"])
    P.dma("sync", gmix[:], dr["gmixT"], writes=["gmix"])
    P.dma("sync", gffn[:], dr["gffnT"], writes=["gffn"])
    P.add("scalar", lambda e: e.activation(out=scl[:], in_=condT[:], func=AF.Silu), reads=["condT"], writes=["scl"])
    for cb in range(6):
        P.dma("sync", brow[:], dr["ada_b"][0:1, cb * 2048:(cb + 1) * 2048].partition_broadcast(2), writes=["brow"])
        for k in range(DC):
            wt, wk = wring.next()
            P.dma("sync" if k % 2 == 0 else "scalar", wt[:], dr["ada_w"][k * 128:(k + 1) * 128, cb * 2048:(cb + 1) * 2048], writes=[wk])
            for q in range(4):
                P.add("tensor", lambda e, wt=wt, q=q, k=k: e.matmul(psT[q][:], scl[:, k, :], wt[:, q * 512:(q + 1) * 512], start=(k == 0), stop=(k == DC - 1)),
                      reads=[wk, "scl"], writes=[f"psT{q}"])
        for q in range(4):
            P.add("vector", lambda e, q=q: e.tensor_tensor(grow_sb[:, q * 512:(q + 1) * 512], psT[q][:], brow[:, q * 512:(q + 1) * 512], ALU.add),
                  reads=[f"psT{q}", "brow"], writes=["grow_sb"])
        P.dma("sync", dr["modrow"][:, cb * 2048:(cb + 1) * 2048], grow_sb[:], reads=["grow_sb"], writes=["modrow"])
    for n in range(2):
        P.dma("sync", modsT[:, :, n], dr["modrow"][n, :].rearrange("(j p) -> p j", p=128), reads=["modrow"], writes=["modsT"],
              allow_slow_non_contiguous=True)
    A1, A2 = pers["A1"], pers["A2"]
    for n in range(2):
        P.add("vector", lambda e, n=n: e.scalar_tensor_tensor(A1[:, :, n], modsT[:, 16:32, n], 1.0, gmix[:], ALU.add, ALU.mult),
              reads=["modsT", "gmix"], writes=["A1"])
        P.add("vector", lambda e, n=n: e.scalar_tensor_tensor(A2[:, :, n], modsT[:, 64:80, n], 1.0, gffn[:], ALU.add, ALU.mult),
              reads=["modsT", "gffn"], writes=["A2"])
    P.fence()
    st.close()


def phase_A(P, nc, dr, pers):
    st = contextlib.ExitStack()
    HT = 17 * 128
    uT = P.sb([128, DC, HT], BF16, "uT", st)
    ident = P.sb([128, 128], BF16, "identA", st)
    pswap = P.sb([128, 128], BF16, "pswapA", st)
    ropeC = P.sb([128, HT], F32, "ropeC", st)
    ropeS = P.sb([128, HT], F32, "ropeS", st)
    stat = P.sb([128, 4], F32, "statA", st)
    junk = P.sb([128, D], BF16, "junkA", st)
    hring = Ring(P, 2, [128, D], F32, "hA", stack=st)
    xring = Ring(P, 2, [128, D], BF16, "xnA", stack=st)
    wfm = Ring(P, 2, [128, DC, 128], BF16, "wfm", stack=st)
    wtm = Ring(P, 2, [128, DC, 512], BF16, "wtm", stack=st)
    abf = Ring(P, 2, [128, 512], BF16, "abf", stack=st)
    t1r = Ring(P, 2, [128, 512], F32, "t1r", stack=st)
    of32 = Ring(P, 3, [128, 512], F32, "of32", stack=st)
    obf = Ring(P, 3, [128, 512], BF16, "obf", stack=st)
    tps = Ring(P, 2, [128, 1024], BF16, "tpsA", psum=True, stack=st)
    pj = Ring(P, 4, [128, 512], F32, "pjA", psum=True, stack=st)
    psw = Ring(P, 2, [128, 512], F32, "pswA", psum=True, stack=st)
    A1, modsT = pers["A1"], pers["modsT"]

    P.dma("gpsimd", ident[:], dr["ident"], writes=["identA"])
    P.dma("gpsimd", pswap[:], dr["pswap"], writes=["pswapA"])
    for half in range(2):
        tok0 = half * HT
        P.dma("sync", ropeC[:], dr["ropeC"][:, tok0:tok0 + HT], writes=["ropeC"])
        P.dma("sync", ropeS[:], dr["ropeS"][:, tok0:tok0 + HT], writes=["ropeS"])
        for tt in range(17):
            gt = half * 17 + tt
            n = 1 if gt < 2 else 0
            ht, hk = hring.next()
            P.dma("sync", ht[:], dr["hin"][gt * 128:(gt + 1) * 128, :], writes=[hk])
            P.add("scalar", lambda e, ht=ht: e.activation(out=junk[:], in_=ht[:], func=AF.Square, accum_out=stat[:, 0:1]),
                  reads=[hk, "statA"], writes=["junkA", "statA"])
            P.add("scalar", lambda e: e.activation(out=stat[:, 1:2], in_=stat[:, 0:1], func=AF.Sqrt, scale=1.0 / D, bias=EPS),
                  reads=["statA"], writes=["statA"])
            P.add("vector", lambda e: e.reciprocal(stat[:, 2:3], stat[:, 1:2]), reads=["statA"], writes=["statA"])
            xn, xk = xring.next()
            P.add("vector", lambda e, xn=xn, ht=ht: e.tensor_scalar(xn[:], ht[:], stat[:, 2:3], None, ALU.mult),
                  reads=[hk, "statA"], writes=[xk])
            P.add("vector", lambda e: e.memset(stat[:, 0:1], 0.0), reads=["statA"], writes=["statA"])
            for hc in range(2):
                tp, tk = tps.next()
                for c8 in range(8):
                    c = hc * 8 + c8
                    P.add("tensor", lambda e, tp=tp, xn=xn, c=c, c8=c8: e.transpose(tp[:, c8 * 128:(c8 + 1) * 128], xn[:, c * 128:(c + 1) * 128], ident[:]),
                          reads=[xk, "identA"], writes=[tk])
                for c8 in range(8):
                    c = hc * 8 + c8
                    dst = uT[:, c, tt * 128:(tt + 1) * 128]
                    src = tp[:, c8 * 128:(c8 + 1) * 128]
                    if c % 2 == 0:
                        P.add("scalar", lambda e, dst=dst, src=src, c=c, n=n: e.activation(out=dst, in_=src, func=AF.Identity, scale=A1[:, c, n:n + 1], bias=modsT[:, c, n:n + 1]),
                              reads=[tk, "A1", "modsT"], writes=[("uT", tt)])
                    else:
                        P.add("vector", lambda e, dst=dst, src=src, c=c, n=n: e.tensor_scalar(dst, src, A1[:, c, n:n + 1], modsT[:, c, n:n + 1], ALU.mult, ALU.add),
                              reads=[tk, "A1", "modsT"], writes=[("uT", tt)])
        if CUT == 1:
            continue
        grps = groups_of(0, HT)
        for m in (range(N_FM) if not os.environ.get("KM") else [int(v) for v in os.environ["KM"].split(",")]):
            wt, wk = wfm.next()
            P.dma("gpsimd", wt[:], dr["w_in_p"][:, m * 128:(m + 1) * 128].rearrange("(c p) n -> p c n", p=128), writes=[wk])
            M = 64 if m == 14 else 128
            for (n0, nsz) in grps:
                pt, pk = pj.next()
                ukeys = [("uT", tt) for tt in range(n0 // 128, (n0 + nsz) // 128)]
                for c in range(DC):
                    P.add("tensor", lambda e, pt=pt, wt=wt, c=c, n0=n0, nsz=nsz, M=M: e.matmul(pt[0:M, 0:nsz], wt[:, c, 0:M], uT[:, c, n0:n0 + nsz], start=(c == 0), stop=(c == DC - 1)),
                          reads=[wk] + ukeys, writes=[pk])
                g0 = tok0 + n0
                if m < 10:
                    RS = int(os.environ.get("ROPE_STEP", "9"))
                    ab, ak = abf.next()
                    P.add("scalar", lambda e, ab=ab, pt=pt, nsz=nsz: e.activation(out=ab[:, 0:nsz], in_=pt[:, 0:nsz], func=AF.Copy), reads=[pk], writes=[ak])
                    dst = dr["qT"][m, :, g0:g0 + nsz] if m < 8 else dr["kT"][m - 8, :, g0:g0 + nsz]
                    if RS == 1:
                        P.dma("sync", dst, ab[:, 0:nsz], reads=[ak])
                        continue
                    sw, sk = psw.next()
                    P.add("tensor", lambda e, sw=sw, ab=ab, nsz=nsz: e.matmul(sw[:, 0:nsz], pswap[:], ab[:, 0:nsz], start=True, stop=True),
                          reads=[ak, "pswapA"], writes=[sk])
                    if RS == 2:
                        ob, ok = obf.next()
                        P.add("scalar", lambda e, ob=ob, sw=sw, nsz=nsz: e.activation(out=ob[:, 0:nsz], in_=sw[:, 0:nsz], func=AF.Copy), reads=[sk], writes=[ok])
                        P.dma("sync", dst, ob[:, 0:nsz], reads=[ok])
                        continue
                    t1, t1k = t1r.next()
                    P.add("vector", lambda e, t1=t1, ab=ab, n0=n0, nsz=nsz: e.tensor_tensor(t1[:, 0:nsz], ab[:, 0:nsz], ropeC[:, n0:n0 + nsz], ALU.mult),
                          reads=[ak, "ropeC"], writes=[t1k])
                    if RS == 3:
                        ob, ok = obf.next()
                        P.add("scalar", lambda e, ob=ob, t1=t1, nsz=nsz: e.activation(out=ob[:, 0:nsz], in_=t1[:, 0:nsz], func=AF.Copy), reads=[t1k], writes=[ok])
                        P.dma("sync", dst, ob[:, 0:nsz], reads=[ok])
                        continue
                    t2, t2k = of32.next()
                    P.add("vector", lambda e, t2=t2, sw=sw, n0=n0, nsz=nsz: e.tensor_tensor(t2[:, 0:nsz], sw[:, 0:nsz], ropeS[:, n0:n0 + nsz], ALU.mult),
                          reads=[sk, "ropeS"], writes=[t2k])
                    ob, ok = obf.next()
                    P.add("vector", lambda e, ob=ob, t1=t1, t2=t2, nsz=nsz: e.tensor_tensor(ob[:, 0:nsz], t1[:, 0:nsz], t2[:, 0:nsz], ALU.add),
                          reads=[t1k, t2k], writes=[ok])
                    P.dma("sync", dst, ob[:, 0:nsz], reads=[ok])
                elif m < 14:
                    ob, ok = obf.next()
                    P.add("scalar", lambda e, ob=ob, pt=pt, nsz=nsz: e.activation(out=ob[:, 0:nsz], in_=pt[:, 0:nsz], func=AF.Copy), reads=[pk], writes=[ok])
                    dst = dr["gqT"][m - 10, :, g0:g0 + nsz] if m < 12 else dr["gkT"][m - 12, :, g0:g0 + nsz]
                    P.dma("sync", dst, ob[:, 0:nsz], reads=[ok])
                else:
                    of, ofk = of32.next()
                    P.add("scalar" if (m % 2 == 0) else "vector",
                          (lambda e, of=of, pt=pt, nsz=nsz, M=M: e.activation(out=of[0:M, 0:nsz], in_=pt[0:M, 0:nsz], func=AF.Copy)) if (m % 2 == 0) else
                          (lambda e, of=of, pt=pt, nsz=nsz, M=M: e.tensor_copy(of[0:M, 0:nsz], pt[0:M, 0:nsz])),
                          reads=[pk], writes=[ofk])
                    if m == 14:
                        dst = dr["gzT"][:, g0:g0 + nsz]
                    elif m < 19:
                        dst = dr["lxT"][m - 15, :, g0:g0 + nsz]
                    else:
                        dst = dr["lgT"][m - 19, :, g0:g0 + nsz]
                    P.dma("sync", dst, of[0:M, 0:nsz], reads=[ofk])
        if CUT == 2:
            continue
        for nb in range(N_TM):
            wt, wk = wtm.next()
            c0 = N_FM * 128 + nb * 512
            P.dma("gpsimd", wt[:], dr["w_in_p"][:, c0:c0 + 512].rearrange("(c p) n -> p c n", p=128), writes=[wk])
            for tt in range(17):
                pt, pk = pj.next()
                for c in range(DC):
                    P.add("tensor", lambda e, pt=pt, wt=wt, c=c, tt=tt: e.matmul(pt[:], uT[:, c, tt * 128:(tt + 1) * 128], wt[:, c, :], start=(c == 0), stop=(c == DC - 1)),
                          reads=[wk, ("uT", tt)], writes=[pk])
                r0 = tok0 + tt * 128
                if nb < 2:
                    ob, ok = obf.next()
                    eng = "scalar" if tt % 2 == 0 else "vector"
                    P.add(eng, (lambda e, ob=ob, pt=pt: e.activation(out=ob[:], in_=pt[:], func=AF.Copy)) if eng == "scalar" else
                          (lambda e, ob=ob, pt=pt: e.tensor_copy(ob[:], pt[:])), reads=[pk], writes=[ok])
                    if nb == 0:
                        P.dma("sync", dr["vS"][r0:r0 + 128, :], ob[:, 0:256], reads=[ok], semkey=("dma", ok, 0))
                        P.dma("sync", dr["gkS"][r0:r0 + 128, :], ob[:, 256:512], reads=[ok], semkey=("dma", ok, 1))
                    else:
                        P.dma("sync", dr["gvS"][r0:r0 + 128, :], ob[:], reads=[ok])
                else:
                    of, ofk = of32.next()
                    eng = "scalar" if tt % 2 == 0 else "vector"
                    P.add(eng, (lambda e, of=of, pt=pt: e.activation(out=of[:], in_=pt[:], func=AF.Copy)) if eng == "scalar" else
                          (lambda e, of=of, pt=pt: e.tensor_copy(of[:], pt[:])), reads=[pk], writes=[ofk])
                    P.dma("sync", dr["grS"][r0:r0 + 128, :], of[:], reads=[ofk])
    P.fence()
    st.close()


_DEINT = np.concatenate([np.arange(0, 128, 2), np.arange(1, 128, 2)])


def _w_in_cols(s):
    Z = -1
    cols = []
    for h in range(8):
        cols.append(0 + h * 128 + _DEINT)
    for h in range(2):
        cols.append(1024 + h * 128 + _DEINT)
    for hp in range(2):
        cols.append(1536 + hp * 128 + np.arange(128))
    for hp in range(2):
        cols.append(1792 + hp * 128 + np.arange(128))
    f = 3072 + np.arange(16)
    bw = 3072 + 16 + np.arange(16)
    if s == 1:
        f, bw = bw, f
    cols.append(np.concatenate([np.full(1, Z), f, np.full(15, Z), np.full(1, Z), bw, np.full(79, Z)]))
    for ct in range(4):
        cols.append(3104 + ct * 128 + np.arange(128))
    for ct in range(4):
        cols.append(3616 + ct * 128 + np.arange(128))
    cols.append(1280 + np.arange(256))
    cols.append(1792 + np.arange(256))
    cols.append(2048 + np.arange(512))
    cols.append(2560 + np.arange(512))
    return np.concatenate(cols)


def _rope_tables(s):
    pos = np.arange(NLAT)
    if s == 1:
        pos = pos[::-1]
    row = (pos // 64).astype(np.float64)
    col = (pos % 64).astype(np.float64)
    inv = 10000.0 ** (-np.arange(32, dtype=np.float64) / 32)
    ang = np.concatenate([row[:, None] * inv, col[:, None] * inv], axis=1)
    C = np.ones((128, T), np.float32)
    S = np.zeros((128, T), np.float32)
    C[:64, NCTX:] = np.cos(ang).T
    C[64:, NCTX:] = np.cos(ang).T
    S[:64, NCTX:] = -np.sin(ang).T
    S[64:, NCTX:] = np.sin(ang).T
    return C, S


_CONST_CACHE = {}


def _consts(s):
    if s in _CONST_CACHE:
        return _CONST_CACHE[s]
    c = {}
    c["ropeC"], c["ropeS"] = _rope_tables(s)
    psw = np.zeros((128, 128), np.float32)
    for m in range(128):
        psw[(m + 64) % 128, m] = 1.0
    c["pswap"] = psw
    c["ident"] = np.eye(128, dtype=np.float32)
    jj = np.arange(128)[:, None]
    ii = np.arange(128)[None, :]
    c["maskL"] = np.tile((jj >= ii).astype(np.float32), (1, 4))
    c["maskR"] = np.tile((jj <= ii).astype(np.float32), (1, 4))
    sidx = np.arange(128)[:, None]
    tidx = np.arange(128)[None, :]
    v = -1.0 / 16.0
    tri = np.zeros((4, 128, 128), np.float32)
    tri[0] = v * (sidx <= tidx)
    tri[1] = v * (sidx > tidx)
    tri[2] = v * (sidx >= tidx)
    tri[3] = v * (sidx < tidx)
    c["tri"] = tri
    gm = np.zeros((2, 128, 128), np.float32)
    gm[0] = (sidx <= tidx)
    gm[1] = (sidx >= tidx)
    c["gmask"] = gm
    _CONST_CACHE[s] = c
    return c


def _fm(vec):
    return np.ascontiguousarray(vec.reshape(DC, 128).T)


def prep_core(inp, l, b, s, h_ctx_b, h_lat_b, shared):
    m = {}
    if s == 1:
        h_ctx_b = h_ctx_b[::-1]
        h_lat_b = h_lat_b[::-1]
    m["hin"] = np.ascontiguousarray(np.concatenate([h_ctx_b, h_lat_b], axis=0))
    cond = np.stack([inp["c"][b], inp["c_ctx"]], axis=0)
    m["condT"] = np.ascontiguousarray(cond.reshape(2, DC, 128).transpose(2, 1, 0))
    m.update(shared[(l, s)])
    return m


def prep_shared(inp, l, s):
    m = {}
    dirs = [0, 1] if s == 0 else [1, 0]
    m["ada_w"] = inp["ada_w"][l]
    m["ada_b"] = inp["ada_b"][l][None, :]
    m["gmixT"] = _fm(inp["norm_mix_g"][l])
    m["gffnT"] = _fm(inp["norm_ffn_g"][l])
    w = inp["w_in"][l]
    wz = np.concatenate([w, np.zeros((D, 1), np.float32)], axis=1)
    m["w_in_p"] = np.ascontiguousarray(wz[:, _w_in_cols(s)])
    m.update(_consts(s))
    m["sink"] = inp["attn_sink"][l][None, :]
    gw = np.zeros((64, 256), np.float32)
    gw[0] = inp["gla_gate_b"][l][dirs[0]]
    gw[1:17] = inp["gla_gate_w2"][l][dirs[0]]
    gw[32] = inp["gla_gate_b"][l][dirs[1]]
    gw[33:49] = inp["gla_gate_w2"][l][dirs[1]]
    m["gw2p"] = gw
    m["gnorm"] = inp["gla_norm_g"][l][None, :]
    cw = inp["lru_conv_w"][l]
    taps = np.zeros((5, 512), np.float32)
    if s == 0:
        taps[0:4] = cw
    else:
        taps[1:5] = cw[::-1]
    m["convw"] = np.ascontiguousarray(taps.reshape(5, 4, 128).transpose(2, 1, 0))
    m["convb"] = np.ascontiguousarray(inp["lru_conv_b"][l].reshape(4, 128).T)
    lw = np.zeros((2, 2, 4, 128, 128), np.float32)
    for ai, nm in enumerate(("lru_wa", "lru_wx")):
        for di in range(2):
            for ct in range(4):
                for k in range(2):
                    lw[ai, di, ct, k * 64:(k + 1) * 64, k * 64:(k + 1) * 64] = inp[nm][l][dirs[di]][2 * ct + k]
    m["lru_w"] = lw
    lb = np.zeros((128, 2, 2, 4), np.float32)
    for ai, nm in enumerate(("lru_ba", "lru_bx")):
        for di in range(2):
            lb[:, ai, di, :] = inp[nm][l][dirs[di]].reshape(4, 128).T
    m["lru_b"] = lb
    ll = np.zeros((128, 2, 4), np.float32)
    for di in range(2):
        ll[:, di, :] = inp["lru_lambda"][l][dirs[di]].reshape(4, 128).T
    m["lru_lam"] = ll
    m["w_out"] = inp["w_out"][l]
    m["wr"] = np.ascontiguousarray(np.concatenate([inp["router_g_w"][l], inp["router_e_w"][l]], axis=1))
    m["br"] = np.concatenate([inp["router_g_b"][l], inp["router_e_b"][l]])[None, :]
    m["moe_w1"] = inp["moe_w1"][l]
    m["moe_w3"] = inp["moe_w3"][l]
    m["moe_w2"] = inp["moe_w2"][l]
    m["gfin"] = inp["final_norm_g"][None, :]
    return m


def build_layer(last, phases=("mods", "A", "attn", "gla", "lru", "C"), debug=()):
    nc = bass.Bass("TRN2", target_bir_lowering=False)
    dr = declare_dram(nc, last, debug, moe=("C" in phases))
    P = Prog(nc)
    pers = {
        "modsT": P.sb([128, 96, 2], F32, "modsT"),
        "A1": P.sb([128, DC, 2], F32, "A1"),
        "A2": P.sb([128, DC, 2], F32, "A2"),
        "G": P.sb([128, 17, 32], F32, "Ggate"),
    }
    if "mods" in phases:
        phase_mods(P, nc, dr, pers)
    if "A" in phases:
        phase_A(P, nc, dr, pers)
    if "attn" in phases:
        phase_attn(P, nc, dr, pers, last)
    if "gla" in phases:
        phase_gla(P, nc, dr, pers, last)
    if "lru" in phases:
        phase_lru(P, nc, dr, pers, last)
    if "C" in phases:
        phase_C(P, nc, dr, pers, last)
    P.emit()
    return nc, P


def phase_attn(P, nc, dr, pers, last):
    st = contextlib.ExitStack()
    KT = P.sb([128, T], BF16, "KT", st)
    QT = P.sb([128, 4, T], BF16, "QT", st)
    VE = P.sb([128, NTT, 128], BF16, "VE", st)
    mL = P.sb([128, 512], BF16, "mL", st)
    mR = P.sb([128, 512], BF16, "mR", st)
    ones_c = P.sb([128, 1], BF16, "ones_c", st)
    ones_r = P.sb([1, 128], F32, "ones_r", st)
    sink = P.sb([1, 8], F32, "sinks", st)
    esrow = P.sb([1, 8, 128], F32, "esrow", st)
    pT = Ring(P, 3, [128, 512], BF16, "pT", stack=st)
    den = Ring(P, 2, [1, 512], F32, "den", stack=st)
    rb = Ring(P, 2, [128, 512], F32, "rbA", stack=st)
    ob = Ring(P, 2, [128, 512], BF16, "obA", stack=st)
    ps_s = Ring(P, 3, [128, 512], F32, "ps_s", psum=True, stack=st)
    ps_o = Ring(P, 2, [128, 512], F32, "ps_o", psum=True, stack=st)
    ps_d = Ring(P, 2, [1, 512], F32, "ps_d", psum=True, stack=st)
    ps_b = Ring(P, 1, [128, 512], F32, "ps_b", psum=True, stack=st)
    scale = 128.0 ** -0.5

    P.dma("gpsimd", mL[:], dr["maskL"], writes=["mL"])
    P.dma("gpsimd", mR[:], dr["maskR"], writes=["mR"])
    P.add("vector", lambda e: e.memset(ones_c[:], 1.0), writes=["ones_c"])
    P.add("vector", lambda e: e.memset(ones_r[:], 1.0), writes=["ones_r"])
    P.dma("sync", sink[:], dr["sink"], writes=["sinks"])
    P.add("scalar", lambda e: e.activation(out=sink[:], in_=sink[:], func=AF.Exp), reads=["sinks"], writes=["sinks"])
    for h in range(8):
        P.add("vector", lambda e, h=h: e.tensor_scalar(esrow[:, h, :], ones_r[:], sink[0:1, h:h + 1], None, ALU.mult),
              reads=["ones_r", "sinks"], writes=["esrow"])
    qtiles = list(range(2, NTT)) + ([] if last else [0, 1])
    for s2 in range(2):
        P.dma("sync", KT[:], dr["kT"][s2], writes=["KT"])
        P.dma("sync", QT[:], dr["qT"][4 * s2:4 * s2 + 4].rearrange("h p t -> p h t"), writes=["QT"])
        P.dma("sync", VE[:], dr["vS"][:, s2 * 128:(s2 + 1) * 128].rearrange("(tt p) c -> p tt c", p=128), writes=["VE"])
        for tq in qtiles:
            if tq >= 2:
                keys = []
                if tq > 2:
                    keys.append((tq - 1, mL, "mL"))
                keys.append((tq, None, None))
                if tq < NTT - 1:
                    keys.append((tq + 1, mR, "mR"))
                keys += [(0, None, None), (1, None, None)]
            else:
                keys = [(0, None, None), (1, None, None)]
            po, pok = ps_o.next()
            pd, pdk = ps_d.next()
            for ki, (kt, mk, mkk) in enumerate(keys):
                pss, psk = ps_s.next()
                P.add("tensor", lambda e, pss=pss, kt=kt, tq=tq: e.matmul(pss[:].rearrange("p (h q) -> p h q", h=4), KT[:, kt * 128:(kt + 1) * 128], QT[:, :, tq * 128:(tq + 1) * 128], start=True, stop=True),
                      reads=["KT", "QT"], writes=[psk])
                pt, ptk = pT.next()
                P.add("scalar", lambda e, pt=pt, pss=pss: e.activation(out=pt[:], in_=pss[:], func=AF.Exp, scale=scale), reads=[psk], writes=[ptk])
                if mk is not None:
                    P.add("vector", lambda e, pt=pt, mk=mk: e.tensor_tensor(pt[:], pt[:], mk[:], ALU.mult), reads=[ptk, mkk], writes=[ptk])
                P.add("tensor", lambda e, po=po, kt=kt, pt=pt, ki=ki, nk=len(keys): e.matmul(po[:], VE[:, kt, :], pt[:], start=(ki == 0), stop=(ki == nk - 1)),
                      reads=["VE", ptk], writes=[pok])
                P.add("tensor", lambda e, pd=pd, pt=pt, ki=ki, nk=len(keys): e.matmul(pd[:], ones_c[:], pt[:], start=(ki == 0), stop=(ki == nk - 1)),
                      reads=["ones_c", ptk], writes=[pdk])
            dn, dnk = den.next()
            P.add("vector", lambda e, dn=dn, pd=pd, s2=s2: e.tensor_tensor(dn[:], pd[:], esrow[:, 4 * s2:4 * s2 + 4, :].rearrange("p h q -> p (h q)"), ALU.add),
                  reads=[pdk, "esrow"], writes=[dnk])
            P.add("vector", lambda e, dn=dn: e.reciprocal(dn[:], dn[:]), reads=[dnk], writes=[dnk])
            pb, pbk = ps_b.next()
            P.add("tensor", lambda e, pb=pb, dn=dn: e.matmul(pb[:], ones_r[:], dn[:], start=True, stop=True), reads=["ones_r", dnk], writes=[pbk])
            r_, rk = rb.next()
            P.add("scalar", lambda e, r_=r_, pb=pb: e.activation(out=r_[:], in_=pb[:], func=AF.Copy), reads=[pbk], writes=[rk])
            o_, ok = ob.next()
            P.add("vector", lambda e, o_=o_, po=po, r_=r_: e.tensor_tensor(o_[:], po[:], r_[:], ALU.mult), reads=[pok, rk], writes=[ok])
            P.dma("sync", dr["mixT"][4 * s2:4 * s2 + 4, :, tq * 128:(tq + 1) * 128].rearrange("h p t -> p h t"),
                  o_[:].rearrange("p (h t) -> p h t", h=4), reads=[ok])
    P.fence()
    st.close()


def phase_gla(P, nc, dr, pers, last):
    st = contextlib.ExitStack()
    gzT = P.sb([64, T], F32, "gzTs", st)
    gw2 = P.sb([64, 256], F32, "gw2s", st)
    tri = P.sb([128, 4, 128], F32, "tris", st)
    gmk = P.sb([128, 2, 128], BF16, "gmks", st)
    gnb = P.sb([128, 128], F32, "gnb", st)
    identb = P.sb([128, 128], BF16, "identG", st)
    gq = P.sb([128, T], BF16, "gqs", st)
    gk = P.sb([128, T], BF16, "gks", st)
    gkt = P.sb([128, NTT, 128], BF16, "gkts", st)
    gv = P.sb([128, NTT, 256], BF16, "gvs", st)
    Oacc = P.sb([128, NTT, 256], F32, "Oacc", st)
    S = P.sb([128, 256], F32, "Sst", st)
    Sbf = P.sb([128, 256], BF16, "Sbf", st)
    stat = P.sb([128, 8], F32, "statG", st)
    junk = P.sb([128, 128], F32, "junkG", st)
    e_r = Ring(P, 2, [128, 128], F32, "e_r", stack=st)
    sp_r = Ring(P, 2, [128, 128], F32, "sp_r", stack=st)
    E1r = Ring(P, 2, [128, 128], F32, "E1r", stack=st)
    E2r = Ring(P, 2, [128, 128], F32, "E2r", stack=st)
    E3r = Ring(P, 2, [128, 128], F32, "E3r", stack=st)
    qtr = Ring(P, 2, [128, 128], BF16, "qtr", stack=st)
    ktr = Ring(P, 2, [128, 128], BF16, "ktr", stack=st)
    khr = Ring(P, 2, [128, 128], BF16, "khr", stack=st)
    atr = Ring(P, 3, [128, 128], BF16, "atr", stack=st)
    grr = Ring(P, 2, [128, 256], F32, "grr", stack=st)
    yr = Ring(P, 2, [128, 256], F32, "yr", stack=st)
    ybr = Ring(P, 2, [128, 256], BF16, "ybr", stack=st)
    otr = Ring(P, 2, [128, 256], BF16, "otr", stack=st)
    ps_z = Ring(P, 1, [128, 128], F32, "ps_z", psum=True, stack=st)
    ps_bT = Ring(P, 1, [128, 128], F32, "ps_bT", psum=True, stack=st)
    ps_r = Ring(P, 1, [128, 128], F32, "ps_r", psum=True, stack=st)
    ps_A = Ring(P, 2, [128, 128], F32, "ps_A", psum=True, stack=st)
    ps_o = Ring(P, 1, [128, 256], F32, "ps_og", psum=True, stack=st)
    ps_U = Ring(P, 1, [128, 256], F32, "ps_U", psum=True, stack=st)
    ps_t = Ring(P, 1, [128, 256], BF16, "ps_tg", psum=True, stack=st)

    P.dma("sync", gzT[:], dr["gzT"], writes=["gzT"])
    P.add("vector", lambda e: e.memset(gzT[0:1, :], 1.0), reads=["gzT"], writes=["gzT"])
    P.add("vector", lambda e: e.memset(gzT[32:33, :], 1.0), reads=["gzT"], writes=["gzT"])
    P.dma("sync", gw2[:], dr["gw2p"], writes=["gw2"])
    P.dma("sync", tri[:], dr["tri"].rearrange("k s t -> s k t"), writes=["tri"])
    P.dma("gpsimd", gmk[:], dr["gmask"].rearrange("k s t -> s k t"), writes=["gmk"])
    P.dma("gpsimd", identb[:], dr["ident"], writes=["identG"])
    P.dma("sync", gnb[:], dr["gnorm"].partition_broadcast(128), writes=["gnb"])
    P.add("vector", lambda e: e.memset(stat[:], 0.0), writes=["statG"])
    fwd_order = list(range(NTT))
    bwd_order = [1, 0] + list(range(NTT - 1, 1, -1))
    for hp in range(2):
        P.dma("sync", gq[:], dr["gqT"][hp], writes=["gq"])
        P.dma("sync", gk[:], dr["gkT"][hp], writes=["gk"])
        P.dma("sync", gkt[:], dr["gkS"][:, hp * 128:(hp + 1) * 128].rearrange("(tt p) c -> p tt c", p=128), writes=["gkt"])
        P.dma("sync", gv[:], dr["gvS"][:, hp * 256:(hp + 1) * 256].rearrange("(tt p) c -> p tt c", p=128), writes=["gv"])
        for d in range(2):
            base = 32 * d
            order = fwd_order if d == 0 else bwd_order
            acol = 127 if d == 0 else 0
            P.add("vector", lambda e: e.memset(S[:], 0.0), reads=["S"], writes=["S"])
            P.add("vector", lambda e: e.memset(Sbf[:], 0.0), reads=["Sbf"], writes=["Sbf"])
            for tt in order:
                tok = slice(tt * 128, (tt + 1) * 128)
                pz, pzk = ps_z.next()
                P.add("tensor", lambda e, pz=pz, tok=tok, base=base, hp=hp: e.matmul(pz[:], gzT[base:base + 17, tok], gw2[base:base + 17, hp * 128:(hp + 1) * 128], start=True, stop=True),
                      reads=["gzT", "gw2"], writes=[pzk])
                ee, eek = e_r.next()
                P.add("scalar", lambda e, ee=ee, pz=pz: e.activation(out=ee[:], in_=pz[:], func=AF.Exp, scale=-1.0), reads=[pzk], writes=[eek])
                sp, spk = sp_r.next()
                P.add("scalar", lambda e, sp=sp, ee=ee: e.activation(out=sp[:], in_=ee[:], func=AF.Ln, bias=1.0), reads=[eek], writes=[spk])
                pb, pbk = ps_bT.next()
                P.add("tensor", lambda e, pb=pb, sp=sp, d=d: e.matmul(pb[:], sp[:], tri[:, 2 * d, :], start=True, stop=True), reads=[spk, "tri"], writes=[pbk])
                pr, prk = ps_r.next()
                P.add("tensor", lambda e, pr=pr, sp=sp, d=d: e.matmul(pr[:], tri[:, 2 * d + 1, :], sp[:], start=True, stop=True), reads=[spk, "tri"], writes=[prk])
                E1, E1k = E1r.next()
                E2, E2k = E2r.next()
                E3, E3k = E3r.next()
                P.add("scalar", lambda e, E1=E1, pb=pb: e.activation(out=E1[:], in_=pb[:], func=AF.Exp), reads=[pbk], writes=[E1k])
                P.add("scalar", lambda e, E2=E2, pb=pb: e.activation(out=E2[:], in_=pb[:], func=AF.Exp, scale=-1.0), reads=[pbk], writes=[E2k])
                P.add("scalar", lambda e, E3=E3, pr=pr: e.activation(out=E3[:], in_=pr[:], func=AF.Exp), reads=[prk], writes=[E3k])
                qt, qtk = qtr.next()
                kt, ktk = ktr.next()
                kh, khk = khr.next()
                P.add("vector", lambda e, qt=qt, E1=E1, tok=tok: e.scalar_tensor_tensor(qt[:], gq[:, tok], 0.125, E1[:], ALU.mult, ALU.mult), reads=["gq", E1k], writes=[qtk])
                P.add("vector", lambda e, kt=kt, E2=E2, tok=tok: e.tensor_tensor(kt[:], gk[:, tok], E2[:], ALU.mult), reads=["gk", E2k], writes=[ktk])
                P.add("vector", lambda e, kh=kh, E3=E3, tt=tt: e.tensor_tensor(kh[:], gkt[:, tt, :], E3[:], ALU.mult), reads=["gkt", E3k], writes=[khk])
                po, pok = ps_o.next()
                for h in range(2):
                    hs = slice(h * 64, (h + 1) * 64)
                    pa, pak = ps_A.next()
                    P.add("tensor", lambda e, pa=pa, kt=kt, qt=qt, hs=hs: e.matmul(pa[:], kt[hs, :], qt[hs, :], start=True, stop=True), reads=[ktk, qtk], writes=[pak])
                    at, atk = atr.next()
                    P.add("vector", lambda e, at=at, pa=pa, d=d: e.tensor_tensor(at[:], pa[:], gmk[:, d, :], ALU.mult), reads=[pak, "gmk"], writes=[atk])
                    P.add("tensor", lambda e, po=po, at=at, tt=tt, h=h: e.matmul(po[:, h * 128:(h + 1) * 128], at[:], gv[:, tt, h * 128:(h + 1) * 128], start=True, stop=False),
                          reads=[atk, "gv"], writes=[pok])
                    P.add("tensor", lambda e, po=po, qt=qt, hs=hs, h=h: e.matmul(po[:, h * 128:(h + 1) * 128], qt[hs, :], Sbf[hs, h * 128:(h + 1) * 128], start=False, stop=True),
                          reads=[qtk, "Sbf"], writes=[pok])
                pu, puk = ps_U.next()
                P.add("tensor", lambda e, pu=pu, kh=kh, tt=tt: e.matmul(pu[:], kh[:], gv[:, tt, :], start=True, stop=True), reads=[khk, "gv"], writes=[puk])
                P.add("vector", lambda e, pu=pu, E1=E1, acol=acol: e.scalar_tensor_tensor(S[:], S[:], E1[:, acol:acol + 1], pu[:], ALU.mult, ALU.add),
                      reads=["S", E1k, puk], writes=["S"])
                P.add("scalar", lambda e: e.activation(out=Sbf[:], in_=S[:], func=AF.Copy), reads=["S"], writes=["Sbf"])
                if d == 0:
                    P.add("scalar", lambda e, po=po, tt=tt: e.activation(out=Oacc[:, tt, :], in_=po[:], func=AF.Copy), reads=[pok], writes=[("Oacc", tt)])
                else:
                    P.add("vector", lambda e, po=po, tt=tt: e.tensor_tensor(Oacc[:, tt, :], po[:], Oacc[:, tt, :], ALU.add), reads=[pok, ("Oacc", tt)], writes=[("Oacc", tt)])
        for tt in (range(2, NTT) if last else range(NTT)):
            g_, gk_ = grr.next()
            P.dma("sync", g_[:], dr["grS"][tt * 128:(tt + 1) * 128, hp * 256:(hp + 1) * 256], writes=[gk_])
            P.add("scalar", lambda e, g_=g_: e.activation(out=g_[:], in_=g_[:], func=AF.Silu), reads=[gk_], writes=[gk_])
            y_, yk = yr.next()
            for h in range(2):
                oh = Oacc[:, tt, h * 128:(h + 1) * 128]
                P.add("scalar", lambda e, oh=oh, h=h: e.activation(out=junk[:], in_=oh, func=AF.Square, accum_out=stat[:, h:h + 1]),
                      reads=[("Oacc", tt), "statG"], writes=["junkG", "statG"])
                P.add("scalar", lambda e, h=h: e.activation(out=stat[:, 2 + h:3 + h], in_=stat[:, h:h + 1], func=AF.Sqrt, scale=1.0 / 128, bias=EPS), reads=["statG"], writes=["statG"])
                P.add("vector", lambda e, h=h: e.reciprocal(stat[:, 4 + h:5 + h], stat[:, 2 + h:3 + h]), reads=["statG"], writes=["statG"])
                P.add("vector", lambda e, y_=y_, oh=oh, h=h: e.scalar_tensor_tensor(y_[:, h * 128:(h + 1) * 128], oh, stat[:, 4 + h:5 + h], gnb[:], ALU.mult, ALU.mult),
                      reads=[("Oacc", tt), "statG", "gnb"], writes=[yk])
            P.add("vector", lambda e: e.memset(stat[:, 0:2], 0.0), reads=["statG"], writes=["statG"])
            yb, ybk = ybr.next()
            P.add("vector", lambda e, yb=yb, y_=y_, g_=g_: e.tensor_tensor(yb[:], y_[:], g_[:], ALU.mult), reads=[yk, gk_], writes=[ybk])
            pt, ptk = ps_t.next()
            for h in range(2):
                P.add("tensor", lambda e, pt=pt, yb=yb, h=h: e.transpose(pt[:, h * 128:(h + 1) * 128], yb[:, h * 128:(h + 1) * 128], identb[:]), reads=[ybk, "identG"], writes=[ptk])
            ot, otk = otr.next()
            P.add("scalar", lambda e, ot=ot, pt=pt: e.activation(out=ot[:], in_=pt[:], func=AF.Copy), reads=[ptk], writes=[otk])
            P.dma("sync", dr["mixT"][8 + 2 * hp:10 + 2 * hp, :, tt * 128:(tt + 1) * 128].rearrange("h p t -> p h t"),
                  ot[:].rearrange("p (h t) -> p h t", h=2), reads=[otk])
    P.fence()
    st.close()


def phase_lru(P, nc, dr, pers, last):
    st = contextlib.ExitStack()
    XW = T + 8
    LAT0 = 262
    XP = P.sb([128, XW], F32, "XP", st)
    xc = P.sb([128, T], F32, "xc", st)
    xcb = P.sb([128, T], BF16, "xcb", st)
    a_all = P.sb([128, T], F32, "a_all", st)
    u_all = P.sb([128, T], F32, "u_all", st)
    hf = P.sb([128, T], F32, "hf", st)
    hb = P.sb([128, T], F32, "hb", st)
    lg = P.sb([128, T], F32, "lgs", st)
    cw = P.sb([128, 4, 5], F32, "cws", st)
    cb = P.sb([128, 4], F32, "cbs", st)
    lw = P.sb([128, 16, 128], BF16, "lws", st)
    lb = P.sb([128, 16], F32, "lbs", st)
    lam = P.sb([128, 8], F32, "lams", st)
    cs = P.sb([128, 8], F32, "css", st)
    tr = Ring(P, 2, [128, 512], F32, "lr_r", stack=st)
    ti = Ring(P, 2, [128, 512], F32, "lr_i", stack=st)
    t2 = Ring(P, 2, [128, 512], F32, "lr_2", stack=st)
    obr = Ring(P, 2, [128, 512], BF16, "lr_o", stack=st)
    ps_a = Ring(P, 2, [128, 512], F32, "ps_la", psum=True, stack=st)
    ps_i = Ring(P, 2, [128, 512], F32, "ps_li", psum=True, stack=st)

    P.dma("sync", cw[:], dr["convw"], writes=["cw"])
    P.dma("sync", cb[:], dr["convb"], writes=["cb"])
    P.dma("gpsimd", lw[:], dr["lru_w"].rearrange("a d c i o -> i (a d c) o"), writes=["lw"])
    P.dma("sync", lb[:], dr["lru_b"].rearrange("p a d c -> p (a d c)"), writes=["lb"])
    P.dma("sync", lam[:], dr["lru_lam"].rearrange("p d c -> p (d c)"), writes=["lam"])
    P.add("scalar", lambda e: e.activation(out=cs[:], in_=lam[:], func=AF.Exp, scale=-1.0), reads=["lam"], writes=["cs"])
    P.add("scalar", lambda e: e.activation(out=cs[:], in_=cs[:], func=AF.Ln, bias=1.0), reads=["cs"], writes=["cs"])
    P.add("vector", lambda e: e.tensor_scalar(cs[:], cs[:], -8.0, None, ALU.mult), reads=["cs"], writes=["cs"])
    P.add("vector", lambda e: e.memset(XP[:], 0.0), writes=["XP"])
    grps = groups_of(0, T)
    for ct in range(4):
        P.dma("sync", XP[:, 2:2 + NCTX], dr["lxT"][ct, :, 0:NCTX], reads=[], writes=["XP"])
        P.dma("sync", XP[:, LAT0:LAT0 + NLAT], dr["lxT"][ct, :, NCTX:T], reads=[], writes=["XP"], semkey=("dma", "XP", 1))
        P.dma("scalar", lg[:], dr["lgT"][ct], writes=["lg"])
        for (o_in, o_out, n) in ((0, 0, NCTX), (LAT0 - 2, NCTX, NLAT)):
            P.add("vector", lambda e, o_in=o_in, o_out=o_out, n=n, ct=ct: e.tensor_scalar(xc[:, o_out:o_out + n], XP[:, o_in:o_in + n], cw[:, ct, 0:1], cb[:, ct:ct + 1], ALU.mult, ALU.add),
                  reads=["XP", "cw", "cb"], writes=["xc"])
            for j in range(1, 5):
                P.add("vector", lambda e, o_in=o_in, o_out=o_out, n=n, ct=ct, j=j: e.scalar_tensor_tensor(xc[:, o_out:o_out + n], XP[:, o_in + j:o_in + j + n], cw[:, ct, j:j + 1], xc[:, o_out:o_out + n], ALU.mult, ALU.add),
                      reads=["XP", "cw", "xc"], writes=["xc"])
        P.add("scalar", lambda e: e.activation(out=xcb[:], in_=xc[:], func=AF.Copy), reads=["xc"], writes=["xcb"])
        for d in range(2):
            ia = (0 * 2 + d) * 4 + ct
            ix = (1 * 2 + d) * 4 + ct
            ic = d * 4 + ct
            for (n0, nsz) in grps:
                pa, pak = ps_a.next()
                pi, pik = ps_i.next()
                P.add("tensor", lambda e, pa=pa, ia=ia, n0=n0, nsz=nsz: e.matmul(pa[:, 0:nsz], lw[:, ia, :], xcb[:, n0:n0 + nsz], start=True, stop=True), reads=["lw", "xcb"], writes=[pak])
                P.add("tensor", lambda e, pi=pi, ix=ix, n0=n0, nsz=nsz: e.matmul(pi[:, 0:nsz], lw[:, ix, :], xcb[:, n0:n0 + nsz], start=True, stop=True), reads=["lw", "xcb"], writes=[pik])
                r_, rk = tr.next()
                i_, ik = ti.next()
                P.add("scalar", lambda e, r_=r_, pa=pa, ia=ia, nsz=nsz: e.activation(out=r_[:, 0:nsz], in_=pa[:, 0:nsz], func=AF.Sigmoid, bias=lb[:, ia:ia + 1]), reads=[pak, "lb"], writes=[rk])
                P.add("scalar", lambda e, i_=i_, pi=pi, ix=ix, nsz=nsz: e.activation(out=i_[:, 0:nsz], in_=pi[:, 0:nsz], func=AF.Sigmoid, bias=lb[:, ix:ix + 1]), reads=[pik, "lb"], writes=[ik])
                P.add("scalar", lambda e, r_=r_, ic=ic, n0=n0, nsz=nsz: e.activation(out=a_all[:, n0:n0 + nsz], in_=r_[:, 0:nsz], func=AF.Exp, scale=cs[:, ic:ic + 1]), reads=[rk, "cs"], writes=["a_all"])
                s_, sk = t2.next()
                P.add("gpsimd", lambda e, s_=s_, n0=n0, nsz=nsz: e.tensor_tensor(s_[:, 0:nsz], a_all[:, n0:n0 + nsz], a_all[:, n0:n0 + nsz], ALU.mult), reads=["a_all"], writes=[sk])
                P.add("scalar", lambda e, s_=s_, nsz=nsz: e.activation(out=s_[:, 0:nsz], in_=s_[:, 0:nsz], func=AF.Sqrt, scale=-1.0, bias=1.0), reads=[sk], writes=[sk])
                P.add("vector", lambda e, i_=i_, n0=n0, nsz=nsz: e.tensor_tensor(i_[:, 0:nsz], i_[:, 0:nsz], xc[:, n0:n0 + nsz], ALU.mult), reads=[ik, "xc"], writes=[ik])
                P.add("vector", lambda e, i_=i_, s_=s_, n0=n0, nsz=nsz: e.tensor_tensor(u_all[:, n0:n0 + nsz], i_[:, 0:nsz], s_[:, 0:nsz], ALU.mult), reads=[ik, sk], writes=["u_all"])
            h_ = hf if d == 0 else hb
            hk = "hf" if d == 0 else "hb"
            if d == 0:
                P.add("vector", lambda e, h_=h_: e.tensor_tensor_scan(h_[:, :], a_all[:, :], u_all[:, :], 0.0, ALU.mult, ALU.add), reads=["a_all", "u_all"], writes=[hk])
            else:
                P.add("vector", lambda e, h_=h_: e.tensor_tensor_scan(h_[:, NCTX - 1::-1] if False else h_[:, 0:NCTX][:, ::-1], a_all[:, 0:NCTX][:, ::-1], u_all[:, 0:NCTX][:, ::-1], 0.0, ALU.mult, ALU.add),
                      reads=["a_all", "u_all"], writes=[hk])
                P.add("vector", lambda e, h_=h_: e.tensor_tensor_scan(h_[:, NCTX:T][:, ::-1], a_all[:, NCTX:T][:, ::-1], u_all[:, NCTX:T][:, ::-1], h_[:, 0:1], ALU.mult, ALU.add),
                      reads=["a_all", "u_all", hk], writes=[hk])
        for (n0, nsz) in grps:
            if last and n0 + nsz <= NCTX:
                continue
            x = lg[:, n0:n0 + nsz]
            p_, pk_ = tr.next()
            P.add("gpsimd", lambda e, p_=p_, x=x, nsz=nsz: e.tensor_tensor(p_[:, 0:nsz], x, x, ALU.mult), reads=["lg"], writes=[pk_])
            P.add("gpsimd", lambda e, p_=p_, nsz=nsz: e.tensor_scalar(p_[:, 0:nsz], p_[:, 0:nsz], 0.044715, 1.0, ALU.mult, ALU.add), reads=[pk_], writes=[pk_])
            P.add("gpsimd", lambda e, p_=p_, x=x, nsz=nsz: e.tensor_tensor(p_[:, 0:nsz], p_[:, 0:nsz], x, ALU.mult), reads=[pk_, "lg"], writes=[pk_])
            P.add("scalar", lambda e, p_=p_, nsz=nsz: e.activation(out=p_[:, 0:nsz], in_=p_[:, 0:nsz], func=AF.Sigmoid, scale=1.5957691216057308), reads=[pk_], writes=[pk_])
            q_, qk_ = ti.next()
            P.add("vector", lambda e, q_=q_, n0=n0, nsz=nsz: e.tensor_tensor(q_[:, 0:nsz], hf[:, n0:n0 + nsz], hb[:, n0:n0 + nsz], ALU.add), reads=["hf", "hb"], writes=[qk_])
            P.add("gpsimd", lambda e, p_=p_, x=x, nsz=nsz: e.tensor_tensor(p_[:, 0:nsz], p_[:, 0:nsz], x, ALU.mult), reads=[pk_, "lg"], writes=[pk_])
            o_, ok_ = obr.next()
            P.add("vector", lambda e, o_=o_, p_=p_, q_=q_, nsz=nsz: e.tensor_tensor(o_[:, 0:nsz], p_[:, 0:nsz], q_[:, 0:nsz], ALU.mult), reads=[pk_, qk_], writes=[ok_])
            P.dma("sync", dr["mixT"][12 + ct, :, n0:n0 + nsz], o_[:, 0:nsz], reads=[ok_])
    P.fence()
    st.close()


def own_tiles(last):
    tiles = []
    if not last:
        tiles.append((0, 1, 0))
    for i in range(OWN_LAT // 128):
        tiles.append((2 + i, 0, OWN_CTX + i * 128))
    return tiles


def phase_C(P, nc, dr, pers, last):
    tiles = own_tiles(last)
    NT = len(tiles)
    A2, modsT = pers["A2"], pers["modsT"]
    G = pers["G"]
    vTs = dr["vTs"]
    st = contextlib.ExitStack()
    wo = P.sb([128, DC, D], BF16, "wo", st)
    wr = P.sb([128, DC, 36], F32, "wrs", st)
    brb = P.sb([128, 36], F32, "brb", st)
    identf = P.sb([128, 128], F32, "identF", st)
    g1b = [P.sb([128, D], F32, f"g1b{n}", st) for n in range(2)]
    stat = P.sb([128, 4], F32, "statC", st)
    junk = P.sb([128, D], BF16, "junkC", st)
    rt = P.sb([128, 96], F32, "rt", st)
    mixr = Ring(P, 2, [128, DC, 128], BF16, "mixr", stack=st)
    hr = Ring(P, 2, [128, D], F32, "hC", stack=st)
    h1r = Ring(P, 2, [128, D], F32, "h1C", stack=st)
    xnr = Ring(P, 2, [128, D], F32, "xnC", stack=st)
    v32r = Ring(P, 2, [128, DC, 128], F32, "v32", stack=st)
    vbr = Ring(P, 2, [128, DC, 128], BF16, "vbr", stack=st)
    pbig = Ring(P, 6, [128, 512], F32, "pbC", psum=True, stack=st)
    prt = Ring(P, 1, [128, 36], F32, "prt", psum=True, stack=st)

    P.dma("gpsimd", wo[:], dr["w_out"].rearrange("(c p) n -> p c n", p=128), writes=["wo"])
    P.dma("sync", wr[:], dr["wr"].rearrange("(c p) n -> p c n", p=128), writes=["wr"])
    P.dma("sync", brb[:], dr["br"].partition_broadcast(128), writes=["brb"])
    P.dma("sync", identf[:], dr["ident"], writes=["identF"])
    for n in range(2):
        P.dma("sync", g1b[n][:], dr["modrow"][n:n + 1, 2 * D:3 * D].partition_broadcast(128), writes=[f"g1b{n}"])
    P.add("vector", lambda e: e.memset(stat[:], 0.0), writes=["statC"])
    for j, (ut, n, row) in enumerate(tiles):
        mt, mk = mixr.next()
        P.dma("sync", mt[:], dr["mixT"][:, :, ut * 128:(ut + 1) * 128].rearrange("k p t -> p k t"), writes=[mk])
        ht, hk = hr.next()
        P.dma("scalar", ht[:], dr["hin"][ut * 128:(ut + 1) * 128, :], writes=[hk])
        h1, h1k = h1r.next()
        for nb in range(4):
            pt, pk = pbig.next()
            for c in range(DC):
                P.add("tensor", lambda e, pt=pt, mt=mt, c=c, nb=nb: e.matmul(pt[:], mt[:, c, :], wo[:, c, nb * 512:(nb + 1) * 512], start=(c == 0), stop=(c == DC - 1)),
                      reads=[mk, "wo"], writes=[pk])
            P.add("vector", lambda e, h1=h1, pt=pt, nb=nb, n=n: e.tensor_tensor(h1[:, nb * 512:(nb + 1) * 512], pt[:], g1b[n][:, nb * 512:(nb + 1) * 512], ALU.mult),
                  reads=[pk, f"g1b{n}"], writes=[h1k])
        P.add("gpsimd", lambda e, h1=h1, ht=ht: e.tensor_tensor(h1[:], h1[:], ht[:], ALU.add), reads=[h1k, hk], writes=[h1k])
        P.dma("sync", dr["h1"][row:row + 128, :], h1[:], reads=[h1k])
        P.add("scalar", lambda e, h1=h1: e.activation(out=junk[:], in_=h1[:], func=AF.Square, accum_out=stat[:, 0:1]), reads=[h1k, "statC"], writes=["junkC", "statC"])
        P.add("scalar", lambda e: e.activation(out=stat[:, 1:2], in_=stat[:, 0:1], func=AF.Sqrt, scale=1.0 / D, bias=EPS), reads=["statC"], writes=["statC"])
        P.add("vector", lambda e: e.reciprocal(stat[:, 2:3], stat[:, 1:2]), reads=["statC"], writes=["statC"])
        xn, xk = xnr.next()
        P.add("vector", lambda e, xn=xn, h1=h1: e.tensor_scalar(xn[:], h1[:], stat[:, 2:3], None, ALU.mult), reads=[h1k, "statC"], writes=[xk])
        P.add("vector", lambda e: e.memset(stat[:, 0:1], 0.0), reads=["statC"], writes=["statC"])
        v32, v32k = v32r.next()
        for q in range(4):
            pt, pk = pbig.next()
            for c4 in range(4):
                c = q * 4 + c4
                P.add("tensor", lambda e, pt=pt, xn=xn, c=c, c4=c4: e.transpose(pt[:, c4 * 128:(c4 + 1) * 128], xn[:, c * 128:(c + 1) * 128], identf[:]), reads=[xk, "identF"], writes=[pk])
            for c4 in range(4):
                c = q * 4 + c4
                src = pt[:, c4 * 128:(c4 + 1) * 128]
                if q % 2 == 0:
                    P.add("scalar", lambda e, v32=v32, src=src, c=c, n=n: e.activation(out=v32[:, c, :], in_=src, func=AF.Identity, scale=A2[:, c, n:n + 1], bias=modsT[:, 48 + c, n:n + 1]),
                          reads=[pk, "A2", "modsT"], writes=[v32k])
                else:
                    P.add("vector", lambda e, v32=v32, src=src, c=c, n=n: e.tensor_scalar(v32[:, c, :], src, A2[:, c, n:n + 1], modsT[:, 48 + c, n:n + 1], ALU.mult, ALU.add),
                          reads=[pk, "A2", "modsT"], writes=[v32k])
        vb, vbk = vbr.next()
        P.add("gpsimd", lambda e, vb=vb, v32=v32: e.tensor_copy(vb[:], v32[:]), reads=[v32k], writes=[vbk])
        P.dma("sync", vTs[:, :, row:row + 128], vb[:], reads=[vbk])
        pr, prk = prt.next()
        for c in range(DC):
            P.add("tensor", lambda e, pr=pr, v32=v32, c=c: e.matmul(pr[:], v32[:, c, :], wr[:, c, :], start=(c == 0), stop=(c == DC - 1)), reads=[v32k, "wr"], writes=[prk])
        lgt = rt[:, 0:36]
        m4, nm4, s4, pg = rt[:, 36:37], rt[:, 37:38], rt[:, 38:39], rt[:, 39:40]
        e4, oh4, ohp = rt[:, 40:44], rt[:, 44:48], rt[:, 48:52]
        les, oh1, msk, oh2 = rt[:, 52:60], rt[:, 60:68], rt[:, 68:76], rt[:, 76:84]
        m1, m2, dl, e2, w1, w2 = rt[:, 84:85], rt[:, 85:86], rt[:, 86:87], rt[:, 87:88], rt[:, 88:89], rt[:, 89:90]
        V = lambda fn, rd=("rt",), wrk=("rt",): P.add("vector", fn, reads=list(rd), writes=list(wrk))
        P.add("vector", lambda e, pr=pr: e.tensor_tensor(lgt, pr[:], brb[:], ALU.add), reads=[prk, "brb", "rt"], writes=["rt"])
        V(lambda e: e.tensor_reduce(m4, rt[:, 0:4], AX.X, ALU.max))
        V(lambda e: e.tensor_scalar(nm4, m4, -1.0, None, ALU.mult))
        V(lambda e: e.memset(s4, 0.0))
        P.add("scalar", lambda e: e.activation(out=e4, in_=rt[:, 0:4], func=AF.Exp, bias=nm4, accum_out=s4), reads=["rt"], writes=["rt"])
        V(lambda e: e.reciprocal(pg, s4))
        V(lambda e: e.tensor_scalar(oh4, rt[:, 0:4], m4, None, ALU.is_equal))
        V(lambda e: e.tensor_scalar(ohp, oh4, pg, None, ALU.mult))
        V(lambda e: e.tensor_scalar(les, rt[:, 4:12], rt[:, 44:45], None, ALU.mult))
        for g in range(1, 4):
            V(lambda e, g=g: e.scalar_tensor_tensor(les, rt[:, 4 + 8 * g:12 + 8 * g], rt[:, 44 + g:45 + g], les, ALU.mult, ALU.add))
        V(lambda e: e.tensor_reduce(m1, les, AX.X, ALU.max))
        V(lambda e: e.tensor_scalar(oh1, les, m1, None, ALU.is_equal))
        V(lambda e: e.scalar_tensor_tensor(msk, oh1, -1e30, les, ALU.mult, ALU.add))
        V(lambda e: e.tensor_reduce(m2, msk, AX.X, ALU.max))
        V(lambda e: e.tensor_scalar(oh2, msk, m2, None, ALU.is_equal))
        V(lambda e: e.tensor_tensor(dl, m2, m1, ALU.subtract))
        P.add("scalar", lambda e: e.activation(out=e2, in_=dl, func=AF.Exp), reads=["rt"], writes=["rt"])
        V(lambda e: e.tensor_scalar(w1, e2, 1.0, None, ALU.add))
        V(lambda e: e.reciprocal(w1, w1))
        V(lambda e: e.tensor_tensor(w2, e2, w1, ALU.mult))
        V(lambda e: e.tensor_scalar(oh1, oh1, w1, None, ALU.mult))
        V(lambda e: e.scalar_tensor_tensor(oh1, oh2, w2, oh1, ALU.mult, ALU.add))
        for g in range(4):
            P.add("vector", lambda e, g=g, j=j: e.tensor_scalar(G[:, j, g * 8:(g + 1) * 8], oh1, rt[:, 48 + g:49 + g], None, ALU.mult), reads=["rt"], writes=["G"])
    P.fence()
    st.close()
    nh0 = (NT + 1) // 2
    halves = [(0, nh0), (nh0, NT)]
    def do_half(j0, j1):
        nt = j1 - j0
        ntok = nt * 128
        row0 = tiles[j0][2]
        st = contextlib.ExitStack()
        vT = P.sb([128, DC, ntok], BF16, "vTh", st)
        yacc = P.sb([128, nt, D], F32, "yacc", st)
        P.dma("sync", vT[:], vTs[:, :, row0:row0 + ntok], writes=["vTh"])
        for t_ in range(nt):
            P.add("gpsimd", lambda e, t_=t_: e.memset(yacc[:, t_, :], 0.0), writes=[("yacc", t_)])
        st2 = contextlib.ExitStack()
        w1r = Ring(P, 2, [128, DC, 256], BF16, "w1r", stack=st2)
        w3r = Ring(P, 2, [128, DC, 256], BF16, "w3r", stack=st2)
        w2r = Ring(P, 2, [128, 2, D], BF16, "w2r", stack=st2)
        aTr = Ring(P, 2, [128, 2, 512], BF16, "aTr", stack=st2)
        sr = Ring(P, 2, [128, 512], F32, "sr", stack=st2)
        ph1 = Ring(P, 2, [128, 512], F32, "ph1", psum=True, stack=st2)
        ph3 = Ring(P, 2, [128, 512], F32, "ph3", psum=True, stack=st2)
        pyr = Ring(P, 2, [128, 1024], F32, "pyr", psum=True, stack=st2)
        grps = groups_of(0, ntok)
        NE = int(os.environ.get("KNE", "32"))
        for ex in range(NE):
            for fb in range(4):
                w1t, w1k = w1r.next()
                w3t, w3k = w3r.next()
                w2t, w2k = w2r.next()
                P.dma("gpsimd", w1t[:], dr["moe_w1"][ex, :, fb * 256:(fb + 1) * 256].rearrange("(c p) f -> p c f", p=128), writes=[w1k])
                P.dma("gpsimd", w3t[:], dr["moe_w3"][ex, :, fb * 256:(fb + 1) * 256].rearrange("(c p) f -> p c f", p=128), writes=[w3k])
                P.dma("gpsimd", w2t[:], dr["moe_w2"][ex, fb * 256:(fb + 1) * 256, :].rearrange("(c p) n -> p c n", p=128), writes=[w2k])
                for (n0, nsz) in grps:
                    aT, aTk = aTr.next()
                    for fc in range(2):
                        p1, p1k = ph1.next()
                        p3, p3k = ph3.next()
                        for c in range(DC):
                            P.add("tensor", lambda e, p1=p1, w1t=w1t, c=c, fc=fc, n0=n0, nsz=nsz: e.matmul(p1[:, 0:nsz], w1t[:, c, fc * 128:(fc + 1) * 128], vT[:, c, n0:n0 + nsz], start=(c == 0), stop=(c == DC - 1)),
                                  reads=[w1k, "vTh"], writes=[p1k])
                        for c in range(DC):
                            P.add("tensor", lambda e, p3=p3, w3t=w3t, c=c, fc=fc, n0=n0, nsz=nsz: e.matmul(p3[:, 0:nsz], w3t[:, c, fc * 128:(fc + 1) * 128], vT[:, c, n0:n0 + nsz], start=(c == 0), stop=(c == DC - 1)),
                                  reads=[w3k, "vTh"], writes=[p3k])
                        s_, sk = sr.next()
                        P.add("scalar", lambda e, s_=s_, p1=p1, nsz=nsz: e.activation(out=s_[:, 0:nsz], in_=p1[:, 0:nsz], func=AF.Silu), reads=[p1k], writes=[sk])
                        P.add("vector", lambda e, aT=aT, s_=s_, p3=p3, fc=fc, nsz=nsz: e.tensor_tensor(aT[:, fc, 0:nsz], p3[:, 0:nsz], s_[:, 0:nsz], ALU.mult), reads=[p3k, sk], writes=[aTk])
                    for tl in range(nsz // 128):
                        t_ = n0 // 128 + tl
                        jg = j0 + t_
                        for dh in range(2):
                            py, pyk = pyr.next()
                            for fc in range(2):
                                for nb in range(2):
                                    P.add("tensor", lambda e, py=py, aT=aT, w2t=w2t, fc=fc, nb=nb, tl=tl, dh=dh: e.matmul(py[:, nb * 512:(nb + 1) * 512], aT[:, fc, tl * 128:(tl + 1) * 128], w2t[:, fc, dh * 1024 + nb * 512:dh * 1024 + (nb + 1) * 512], start=(fc == 0), stop=(fc == 1)),
                                          reads=[aTk, w2k], writes=[pyk])
                            P.add("vector", lambda e, py=py, t_=t_, dh=dh, jg=jg, ex=ex: e.scalar_tensor_tensor(yacc[:, t_, dh * 1024:(dh + 1) * 1024], py[:], G[:, jg, ex:ex + 1], yacc[:, t_, dh * 1024:(dh + 1) * 1024], ALU.mult, ALU.add),
                                  reads=[pyk, "G", ("yacc", t_)], writes=[("yacc", t_)])
        P.fence()
        st2.close()
        st3 = contextlib.ExitStack()
        g2b = [P.sb([128, D], F32, f"g2b{n}", st3) for n in range(2)]
        gfb = P.sb([128, D], F32, "gfb", st3)
        stat2 = P.sb([128, 4], F32, "statF", st3)
        junk2 = P.sb([128, D], BF16, "junkF", st3)
        h1r2 = Ring(P, 2, [128, D], F32, "h1F", stack=st3)
        for n in range(2):
            P.dma("sync", g2b[n][:], dr["modrow"][n:n + 1, 5 * D:6 * D].partition_broadcast(128), writes=[f"g2b{n}"])
        P.dma("sync", gfb[:], dr["gfin"].partition_broadcast(128), writes=["gfb"])
        P.add("vector", lambda e: e.memset(stat2[:], 0.0), writes=["statF"])
        for t_ in range(nt):
            ut, n, row = tiles[j0 + t_]
            h1, h1k = h1r2.next()
            P.dma("sync", h1[:], dr["h1"][row:row + 128, :], writes=[h1k])
            P.add("vector", lambda e, t_=t_, n=n: e.tensor_tensor(yacc[:, t_, :], yacc[:, t_, :], g2b[n][:], ALU.mult), reads=[("yacc", t_), f"g2b{n}"], writes=[("yacc", t_)])
            P.add("gpsimd", lambda e, h1=h1, t_=t_: e.tensor_tensor(h1[:], h1[:], yacc[:, t_, :], ALU.add), reads=[h1k, ("yacc", t_)], writes=[h1k])
            if last:
                P.add("scalar", lambda e, h1=h1: e.activation(out=junk2[:], in_=h1[:], func=AF.Square, accum_out=stat2[:, 0:1]), reads=[h1k, "statF"], writes=["junkF", "statF"])
                P.add("scalar", lambda e: e.activation(out=stat2[:, 1:2], in_=stat2[:, 0:1], func=AF.Sqrt, scale=1.0 / D, bias=EPS), reads=["statF"], writes=["statF"])
                P.add("vector", lambda e: e.reciprocal(stat2[:, 2:3], stat2[:, 1:2]), reads=["statF"], writes=["statF"])
                P.add("vector", lambda e, h1=h1: e.scalar_tensor_tensor(h1[:], h1[:], stat2[:, 2:3], gfb[:], ALU.mult, ALU.mult), reads=[h1k, "statF", "gfb"], writes=[h1k])
                P.add("vector", lambda e: e.memset(stat2[:, 0:1], 0.0), reads=["statF"], writes=["statF"])
                P.dma("sync", dr["out"][row - OWN_CTX:row - OWN_CTX + 128, :], h1[:], reads=[h1k])
            else:
                P.dma("sync", dr["out"][row:row + 128, :], h1[:], reads=[h1k])
        P.fence()
        st3.close()
        st.close()

    for (j0, j1) in halves:
        do_half(j0, j1)


_NC_CACHE = {}


def _get_nc(last):
    if last not in _NC_CACHE:
        _NC_CACHE[last] = build_layer(last)[0]
    return _NC_CACHE[last]


def kernel(**inputs):
    inp = {k: np.ascontiguousarray(np.asarray(v), dtype=np.float32) for k, v in inputs.items()}
    B = inp["x"].shape[0]
    h_lat = inp["x"]
    h_ctx = inp["ctx"]
    out = None
    for l in range(2):
        last = (l == 1)
        nc = _get_nc(last)
        shared = {(l, s): prep_shared(inp, l, s) for s in (0, 1)}
        maps = [prep_core(inp, l, c // 2, c % 2, h_ctx[c // 2], h_lat[c // 2], shared) for c in range(2 * B)]
        res = run_bass_kernel_spmd(nc, maps, core_ids=list(range(2 * B)))
        if not last:
            new_lat = np.empty_like(h_lat)
            new_ctx = np.empty_like(h_ctx)
            for c in range(2 * B):
                b, s = divmod(c, 2)
                o = np.asarray(res.results[c]["out"])
                if s == 0:
                    new_ctx[b, 0:OWN_CTX] = o[0:OWN_CTX]
                    new_lat[b, 0:OWN_LAT] = o[OWN_CTX:]
                else:
                    new_ctx[b, NCTX - OWN_CTX:] = o[0:OWN_CTX][::-1]
                    new_lat[b, NLAT - OWN_LAT:] = o[OWN_CTX:][::-1]
            h_lat, h_ctx = new_lat, new_ctx
        else:
            out = np.empty_like(h_lat)
            for c in range(2 * B):
                b, s = divmod(c, 2)
                o = np.asarray(res.results[c]["out"])
                if s == 0:
                    out[b, 0:OWN_LAT] = o
                else:
                    out[b, NLAT - OWN_LAT:] = o[::-1]
    return out
```
